# Optimizing a Trainium2 kernel written in Bass

```python
import math
import jax, jax.numpy as jnp
from jax import lax
import numpy as np

D_MODEL = 1024
BATCH = 4
SEQ = 8192
DEPTH = 1

GRID_W = 64
CTX_LEN = 256
HEAD_DIM = 64
A_HEADS = 4
R_HEADS = 4
R_DK = 64
R_DV = 128
A_QK = A_HEADS * 2 * HEAD_DIM
A_V = A_HEADS * 2 * HEAD_DIM
R_QK = R_HEADS * R_DK
R_V = R_HEADS * R_DV
D_IN = 2 * A_QK + A_V + 2 * R_QK + 2 * R_V
D_MIX = A_V + R_V
Q_BLOCK = 128
RET_CHUNK = 128
ROPE_BASE = 10000.0
N_EXPERTS = 64
N_GROUPS = 8
TOPK_GROUPS = 4
TOP_K = 8
D_EXPERT = D_MODEL // 4
D_SHARED = D_MODEL // 4
ROUTED_SCALE = 2.5
MOE_BLOCK = 128
NORM_EPS = 1e-6

kernel_name = "hybrid_diffattn_retention_moe_dit_layer"


def rmsnorm(x, g):
    xf = x.astype(jnp.float32)
    y = xf * lax.rsqrt(jnp.mean(xf * xf, axis=-1, keepdims=True) + NORM_EPS)
    return (y * g.astype(jnp.float32)).astype(x.dtype)


def modulate(h, shift, scale):
    return h * (1 + scale[:, None]) + shift[:, None]


def axial_rope_tables(n_tokens):
    n_rows = n_tokens // GRID_W
    rows = jnp.broadcast_to(jnp.arange(n_rows, dtype=jnp.float32)[:, None], (n_rows, GRID_W)).reshape(-1)
    cols = jnp.broadcast_to(jnp.arange(GRID_W, dtype=jnp.float32)[None, :], (n_rows, GRID_W)).reshape(-1)
    n_freq = HEAD_DIM // 4
    inv_freq = ROPE_BASE ** (-jnp.arange(n_freq, dtype=jnp.float32) / n_freq)
    ang = jnp.stack([rows[:, None] * inv_freq, cols[:, None] * inv_freq], axis=1)
    return jnp.cos(ang), jnp.sin(ang)


def apply_axial_rope(x, cos, sin):
    n_freq = HEAD_DIM // 4
    xr = x.reshape(x.shape[:-1] + (2, 2, n_freq)).astype(jnp.float32)
    shp = (cos.shape[0],) + (1,) * (x.ndim - 3) + (2, n_freq)
    c = cos.reshape(shp)
    s = sin.reshape(shp)
    x1, x2 = xr[..., 0, :], xr[..., 1, :]
    out = jnp.stack([x1 * c - x2 * s, x1 * s + x2 * c], axis=-2)
    return out.reshape(x.shape).astype(x.dtype)


def project_heads(h, w_in):
    bn, t, _ = h.shape
    p = h @ w_in
    sizes = (A_QK, A_QK, A_V, R_QK, R_QK, R_V, R_V)
    cuts = [sum(sizes[:i + 1]) for i in range(len(sizes) - 1)]
    dq, dk, dv, rq, rk, rv, rg = jnp.split(p, cuts, axis=-1)
    return (dq.reshape(bn, t, A_HEADS, 2, HEAD_DIM),
            dk.reshape(bn, t, A_HEADS, 2, HEAD_DIM),
            dv.reshape(bn, t, A_HEADS, 2 * HEAD_DIM),
            rq.reshape(bn, t, R_HEADS, R_DK),
            rk.reshape(bn, t, R_HEADS, R_DK),
            rv.reshape(bn, t, R_HEADS, R_DV),
            rg)


def diff_attend(q, k, v, lam):
    s = jnp.einsum('bqhmd,bkhmd->bhmqk', q, k).astype(jnp.float32)
    p = jax.nn.softmax(s, axis=-1)
    a = p[:, :, 0] - lam * p[:, :, 1]
    return jnp.einsum('bhqk,bkhe->bqhe', a.astype(v.dtype), v)


def retention_chunked(q, k, v, log_gamma, state0):
    b, t, h, dk = q.shape
    dv = v.shape[-1]
    n = t // RET_CHUNK
    f32 = jnp.float32
    qc = q.astype(f32).reshape(b, n, RET_CHUNK, h, dk)
    kc = k.astype(f32).reshape(b, n, RET_CHUNK, h, dk)
    vc = v.astype(f32).reshape(b, n, RET_CHUNK, h, dv)
    lg = log_gamma.astype(f32)
    pos = jnp.arange(RET_CHUNK, dtype=f32)
    rel = pos[:, None] - pos[None, :]
    decay = jnp.where(rel[None] >= 0, jnp.exp(jnp.maximum(rel, 0.0)[None] * lg[:, None, None]), 0.0)
    scores = jnp.einsum('bnihd,bnjhd->bnhij', qc, kc) * decay
    intra = jnp.einsum('bnhij,bnjhe->bnihe', scores, vc)
    k_dec = kc * jnp.exp((RET_CHUNK - 1 - pos)[:, None] * lg)[:, :, None]
    chunk_kv = jnp.einsum('bnjhd,bnjhe->nbhde', k_dec, vc)
    g_chunk = jnp.exp(RET_CHUNK * lg)[None, :, None, None]

    def step(s, kv_i):
        return g_chunk * s + kv_i, s

    state_final, state_prev = lax.scan(step, state0.astype(f32), chunk_kv)
    q_dec = qc * jnp.exp((pos + 1)[:, None] * lg)[:, :, None]
    cross = jnp.einsum('bnihd,nbhde->bnihe', q_dec, state_prev)
    out = (intra + cross).reshape(b, t, h, dv)
    return out.astype(v.dtype), state_final


def bidir_retention(q_c, k_c, v_c, q_x, k_x, v_x, logg_f, logg_b):
    b = q_x.shape[0]
    s0 = jnp.zeros((b, R_HEADS, R_DK, R_DV), jnp.float32)
    flip = lambda a: jnp.flip(a, axis=1)
    o_cf, s_cf = retention_chunked(q_c, k_c, v_c, logg_f, s0)
    o_xf, _ = retention_chunked(q_x, k_x, v_x, logg_f, s_cf)
    o_cb, s_cb = retention_chunked(flip(q_c), flip(k_c), flip(v_c), logg_b, s0)
    o_xb, _ = retention_chunked(flip(q_x), flip(k_x), flip(v_x), logg_b, s_cb)
    return o_cf + flip(o_cb), o_xf + flip(o_xb)


def mixer_merge(att, ret, gate, lam_init, dattn_norm_g, ret_norm_g, w_out):
    bn, t = att.shape[:2]
    a = (rmsnorm(att, dattn_norm_g) * (1 - lam_init)).reshape(bn, t, A_V)
    r = jax.nn.silu(gate) * rmsnorm(ret, ret_norm_g).reshape(bn, t, R_V)
    return jnp.concatenate([a, r], axis=-1) @ w_out


def moe_ffn(h, router_w, router_bias, w1, w3, w2, sw1, sw3, sw2):
    t, d = h.shape
    scores = jax.nn.sigmoid((h @ router_w).astype(jnp.float32))
    sel = scores + router_bias.astype(jnp.float32)
    grp = sel.reshape(t, N_GROUPS, N_EXPERTS // N_GROUPS)
    grp_score = lax.top_k(grp, 2)[0].sum(-1)
    _, top_g = lax.top_k(grp_score, TOPK_GROUPS)
    gmask = jax.nn.one_hot(top_g, N_GROUPS, dtype=jnp.float32).sum(axis=1) > 0
    emask = jnp.repeat(gmask, N_EXPERTS // N_GROUPS, axis=1)
    _, top_e = lax.top_k(jnp.where(emask, sel, -jnp.inf), TOP_K)
    wts = jnp.take_along_axis(scores, top_e, axis=1)
    wts = wts / jnp.sum(wts, axis=-1, keepdims=True) * ROUTED_SCALE
    n_assign = t * TOP_K
    e_flat = top_e.reshape(n_assign)
    t_flat = jnp.repeat(jnp.arange(t, dtype=jnp.int32), TOP_K)
    w_flat = wts.reshape(n_assign)
    order = jnp.argsort(e_flat)
    e_s, t_s, w_s = e_flat[order], t_flat[order], w_flat[order]
    counts = jax.ops.segment_sum(jnp.ones((n_assign,), jnp.int32), e_flat, num_segments=N_EXPERTS)
    padded = (counts + MOE_BLOCK - 1) // MOE_BLOCK * MOE_BLOCK
    pad_end = jnp.cumsum(padded)
    pad_start = pad_end - padded
    start = jnp.cumsum(counts) - counts
    dest = pad_start[e_s] + jnp.arange(n_assign, dtype=jnp.int32) - start[e_s]
    n_pad = -(-n_assign // MOE_BLOCK) * MOE_BLOCK + N_EXPERTS * MOE_BLOCK
    n_blk = n_pad // MOE_BLOCK
    tok_buf = jnp.full((n_pad,), t, jnp.int32).at[dest].set(t_s)
    w_buf = jnp.zeros((n_pad,), jnp.float32).at[dest].set(w_s)
    blk_exp = jnp.clip(jnp.searchsorted(pad_end, jnp.arange(n_blk, dtype=jnp.int32) * MOE_BLOCK, side='right'),
                       0, N_EXPERTS - 1).astype(jnp.int32)
    h_pad = jnp.concatenate([h, jnp.zeros((1, d), h.dtype)], axis=0)

    def body(acc, inp):
        tok, wt, e = inp
        xb = h_pad[tok]
        yb = (jax.nn.silu(xb @ w1[e]) * (xb @ w3[e])) @ w2[e]
        return acc.at[tok].add((yb * wt[:, None].astype(yb.dtype)).astype(acc.dtype)), None

    acc0 = jnp.zeros((t + 1, d), h.dtype)
    acc, _ = lax.scan(body, acc0, (tok_buf.reshape(n_blk, MOE_BLOCK), w_buf.reshape(n_blk, MOE_BLOCK), blk_exp))
    shared = (jax.nn.silu(h @ sw1) * (h @ sw3)) @ sw2
    return acc[:t] + shared


def hybrid_layer(x, xc, mod_x, mod_c, cos, sin, lam_init, update_ctx,
                 norm1_g, w_in, lambda_q1, lambda_k1, lambda_q2, lambda_k2, dattn_norm_g,
                 ret_decay_fwd, ret_decay_bwd, ret_norm_g, w_out,
                 norm2_g, router_w, router_bias, exp_w1, exp_w3, exp_w2,
                 shared_w1, shared_w3, shared_w2):
    b, s, d = x.shape
    sh1, sc1, g1, sh2, sc2, g2 = jnp.split(mod_x, 6, axis=-1)
    csh1, csc1, cg1, csh2, csc2, cg2 = jnp.split(mod_c, 6, axis=-1)

    hx = modulate(rmsnorm(x, norm1_g), sh1, sc1)
    hc = modulate(rmsnorm(xc, norm1_g), csh1, csc1)
    dq_x, dk_x, dv_x, rq_x, rk_x, rv_x, rg_x = project_heads(hx, w_in)
    dq_c, dk_c, dv_c, rq_c, rk_c, rv_c, rg_c = project_heads(hc, w_in)

    dq_x = apply_axial_rope(dq_x, cos, sin) * HEAD_DIM ** -0.5
    dk_x = apply_axial_rope(dk_x, cos, sin)
    rq_x = apply_axial_rope(rq_x, cos, sin)
    rk_x = apply_axial_rope(rk_x, cos, sin) * R_DK ** -0.5
    dq_c = dq_c * HEAD_DIM ** -0.5
    rk_c = rk_c * R_DK ** -0.5

    lam = (jnp.exp(jnp.sum(lambda_q1 * lambda_k1)) - jnp.exp(jnp.sum(lambda_q2 * lambda_k2))).astype(jnp.float32) + lam_init

    k_all = jnp.concatenate([dk_c, dk_x], axis=1)
    v_all = jnp.concatenate([dv_c, dv_x], axis=1)
    n_qblk = s // Q_BLOCK
    q_blocks = dq_x.reshape(b, n_qblk, Q_BLOCK, A_HEADS, 2, HEAD_DIM).swapaxes(0, 1)
    att_x = lax.map(lambda qb: diff_attend(qb, k_all, v_all, lam), q_blocks)
    att_x = att_x.swapaxes(0, 1).reshape(b, s, A_HEADS, 2 * HEAD_DIM)

    logg_f = jax.nn.log_sigmoid(ret_decay_fwd.astype(jnp.float32))
    logg_b = jax.nn.log_sigmoid(ret_decay_bwd.astype(jnp.float32))
    ret_c, ret_x = bidir_retention(rq_c, rk_c, rv_c, rq_x, rk_x, rv_x, logg_f, logg_b)

    y_x = mixer_merge(att_x, ret_x, rg_x, lam_init, dattn_norm_g, ret_norm_g, w_out)
    x = x + g1[:, None] * y_x
    h2 = modulate(rmsnorm(x, norm2_g), sh2, sc2)
    ffn_x = moe_ffn(h2.reshape(b * s, d), router_w, router_bias, exp_w1, exp_w3, exp_w2,
                    shared_w1, shared_w3, shared_w2).reshape(b, s, d)
    x = x + g2[:, None] * ffn_x

    if update_ctx:
        att_c = diff_attend(dq_c, dk_c, dv_c, lam)
        y_c = mixer_merge(att_c, ret_c, rg_c, lam_init, dattn_norm_g, ret_norm_g, w_out)
        xc = xc + cg1[:, None] * y_c
        bc, lc, _ = xc.shape
        h2c = modulate(rmsnorm(xc, norm2_g), csh2, csc2)
        ffn_c = moe_ffn(h2c.reshape(bc * lc, d), router_w, router_bias, exp_w1, exp_w3, exp_w2,
                        shared_w1, shared_w3, shared_w2).reshape(bc, lc, d)
        xc = xc + cg2[:, None] * ffn_c
    else:
        xc = None
    return x, xc


def setup_inputs(seed: int = 0) -> dict:
    key = jax.random.key(seed)
    ks = jax.random.split(key, 32)
    f32 = jnp.float32

    def nrm(k, shape, scale):
        return jax.random.normal(k, shape, f32) * scale

    gamma0 = 1.0 - 2.0 ** (-5.0 - jnp.arange(R_HEADS, dtype=f32))
    logit0 = jnp.log(gamma0) - jnp.log1p(-gamma0)
    return {
        "x": nrm(ks[0], (BATCH, SEQ, D_MODEL), 1.0),
        "c": nrm(ks[1], (BATCH, D_MODEL), 1.0),
        "ctx": nrm(ks[2], (BATCH, CTX_LEN, D_MODEL), 1.0),
        "c_ctx": nrm(ks[3], (D_MODEL,), 1.0),
        "ada_w": nrm(ks[4], (DEPTH, D_MODEL, 6 * D_MODEL), 0.5 * D_MODEL ** -0.5),
        "ada_b": nrm(ks[5], (DEPTH, 6 * D_MODEL), 0.02),
        "norm1_g": 1.0 + nrm(ks[6], (DEPTH, D_MODEL), 0.05),
        "w_in": nrm(ks[7], (DEPTH, D_MODEL, D_IN), D_MODEL ** -0.5),
        "lambda_q1": nrm(ks[8], (DEPTH, HEAD_DIM), 0.1),
        "lambda_k1": nrm(ks[9], (DEPTH, HEAD_DIM), 0.1),
        "lambda_q2": nrm(ks[10], (DEPTH, HEAD_DIM), 0.1),
        "lambda_k2": nrm(ks[11], (DEPTH, HEAD_DIM), 0.1),
        "dattn_norm_g": 1.0 + nrm(ks[12], (DEPTH, 2 * HEAD_DIM), 0.05),
        "ret_decay_fwd": logit0[None] + nrm(ks[13], (DEPTH, R_HEADS), 0.1),
        "ret_decay_bwd": logit0[None] + nrm(ks[14], (DEPTH, R_HEADS), 0.1),
        "ret_norm_g": 1.0 + nrm(ks[15], (DEPTH, R_DV), 0.05),
        "w_out": nrm(ks[16], (DEPTH, D_MIX, D_MODEL), D_MIX ** -0.5),
        "norm2_g": 1.0 + nrm(ks[17], (DEPTH, D_MODEL), 0.05),
        "router_w": nrm(ks[18], (DEPTH, D_MODEL, N_EXPERTS), D_MODEL ** -0.5),
        "router_bias": nrm(ks[19], (DEPTH, N_EXPERTS), 0.01),
        "exp_w1": nrm(ks[20], (DEPTH, N_EXPERTS, D_MODEL, D_EXPERT), D_MODEL ** -0.5),
        "exp_w3": nrm(ks[21], (DEPTH, N_EXPERTS, D_MODEL, D_EXPERT), D_MODEL ** -0.5),
        "exp_w2": nrm(ks[22], (DEPTH, N_EXPERTS, D_EXPERT, D_MODEL), D_EXPERT ** -0.5),
        "shared_w1": nrm(ks[23], (DEPTH, D_MODEL, D_SHARED), D_MODEL ** -0.5),
        "shared_w3": nrm(ks[24], (DEPTH, D_MODEL, D_SHARED), D_MODEL ** -0.5),
        "shared_w2": nrm(ks[25], (DEPTH, D_SHARED, D_MODEL), D_SHARED ** -0.5),
        "final_norm_g": 1.0 + nrm(ks[26], (D_MODEL,), 0.05),
    }


def reference(x, c, ctx, c_ctx, ada_w, ada_b, norm1_g, w_in, lambda_q1, lambda_k1, lambda_q2, lambda_k2,
              dattn_norm_g, ret_decay_fwd, ret_decay_bwd, ret_norm_g, w_out, norm2_g, router_w, router_bias,
              exp_w1, exp_w3, exp_w2, shared_w1, shared_w3, shared_w2, final_norm_g):
    s = x.shape[1]
    cos, sin = axial_rope_tables(s)
    xc = ctx
    for l in range(DEPTH):
        lam_init = 0.8 - 0.6 * math.exp(-0.3 * l)
        mod_x = jax.nn.silu(c) @ ada_w[l] + ada_b[l]
        mod_c = (jax.nn.silu(c_ctx) @ ada_w[l] + ada_b[l])[None]
        x, xc = hybrid_layer(x, xc, mod_x, mod_c, cos, sin, lam_init, l < DEPTH - 1,
                             norm1_g[l], w_in[l], lambda_q1[l], lambda_k1[l], lambda_q2[l], lambda_k2[l],
                             dattn_norm_g[l], ret_decay_fwd[l], ret_decay_bwd[l], ret_norm_g[l], w_out[l],
                             norm2_g[l], router_w[l], router_bias[l], exp_w1[l], exp_w3[l], exp_w2[l],
                             shared_w1[l], shared_w3[l], shared_w2[l])
    return rmsnorm(x, final_norm_g)
```

```python
import contextlib
import math
import numpy as np
import ml_dtypes
import concourse.bass as bass
import concourse.mybir as mybir
from concourse.bass_utils import run_bass_kernel_spmd

F32 = mybir.dt.float32
BF16 = mybir.dt.bfloat16
ALU = mybir.AluOpType
AF = mybir.ActivationFunctionType
AX = mybir.AxisListType

D = 1024
SEQ = 8192
HALF = 4096
CTX = 256
NTOK = CTX + SEQ
NKC = NTOK // 128
NQB = HALF // 512
NE = 64
BIG = 1.0e6
LN8 = math.log(0.125)
EPS = 1e-6


class Prog:
    CE = ("act", "pool", "pe", "dve")
    NDMA = 32

    def __init__(self, nc):
        self.nc = nc
        self.ops = {e: [] for e in ("sync",) + self.CE}
        self.cnt = {e: 0 for e in self.CE}
        self.seen = {e: {} for e in ("sync",) + self.CE}
        self.res = {}
        self.dma_cnt = [0] * self.NDMA
        self.dma_rr = 0
        self.dma_rr_q = {}

    def _deps(self, eng, reads, writes):
        toks = {}

        def add(t):
            if t is None:
                return
            k, v = t
            if eng == "pe" and k == "pe":
                return
            if toks.get(k, 0) < v:
                toks[k] = v
        for r in reads:
            st = self.res.get(r)
            if st is not None:
                add(st["w"])
        for w in writes:
            st = self.res.get(w)
            if st is not None:
                add(st["w"])
                for k, v in st["r"].items():
                    add((k, v))
        out = []
        for k, v in toks.items():
            if self.seen[eng].get(k, 0) < v:
                self.seen[eng][k] = v
                out.append((k, v))
        return out

    def _commit(self, tok, reads, writes):
        k, v = tok
        for r in reads:
            st = self.res.setdefault(r, {"w": None, "r": {}})
            if st["r"].get(k, 0) < v:
                st["r"][k] = v
        for w in writes:
            self.res[w] = {"w": tok, "r": {}}

    def op(self, eng, fn, reads=(), writes=()):
        waits = self._deps(eng, reads, writes)
        self.cnt[eng] += 1
        tok = (eng, self.cnt[eng])
        self.ops[eng].append((waits, fn, eng, 1))
        self._commit(tok, reads, writes)

    def dma(self, fn, reads=(), writes=(), eng="sync"):
        half = self.NDMA // 2
        rr = self.dma_rr_q.setdefault(eng, 0)
        self.dma_rr_q[eng] = (rr + 1) % half
        s = rr + (0 if eng == "sync" else half)
        key = ("dma", s)
        waits = self._deps(eng, reads, writes)
        prev = self.dma_cnt[s]
        if prev > 0 and self.seen[eng].get(key, 0) < prev:
            self.seen[eng][key] = prev
            waits.append((key, prev))
        self.dma_cnt[s] = prev + 16
        tok = (key, prev + 16)
        self.ops[eng].append((waits, fn, key, 16))
        self._commit(tok, reads, writes)

    def barrier(self):
        allw = []
        for s in range(self.NDMA):
            if self.dma_cnt[s] > 0:
                allw.append((("dma", s), self.dma_cnt[s]))
        for e in self.CE:
            if self.cnt[e] > 0:
                allw.append((e, self.cnt[e]))
        for eng in ("sync",) + self.CE:
            waits = []
            for k, v in allw:
                if self.seen[eng].get(k, 0) < v:
                    self.seen[eng][k] = v
                    waits.append((k, v))
            if waits:
                self.ops[eng].append((waits, None, None, 0))
        self.res = {}

    def emit(self):
        nc = self.nc
        with contextlib.ExitStack() as st:
            sems = {}
            for e in self.CE:
                sems[e] = st.enter_context(nc.semaphore("s_" + e))
            for s in range(self.NDMA):
                sems[("dma", s)] = st.enter_context(nc.semaphore("s_dma%d" % s))
            block = st.enter_context(nc.Block())

            def run(engname):
                def body(eng):
                    for waits, fn, inc, amt in self.ops[engname]:
                        for k, v in waits:
                            eng.wait_ge(sems[k], v)
                        if fn is not None:
                            ins = fn(eng)
                            ins.then_inc(sems[inc], amt)
                return body
            block.sync(run("sync"))
            block.scalar(run("act"))
            block.gpsimd(run("pool"))
            block.tensor(run("pe"))
            block.vector(run("dve"))


class Arena:
    def __init__(self, ap, nwords):
        self.ap = ap
        self.n = nwords
        self.off = 0

    def reset(self):
        self.off = 0

    def f32(self, n):
        a = self.ap[:, self.off:self.off + n]
        self.off += n
        assert self.off <= self.n, ("arena overflow", self.off)
        return a

    def bf16(self, n):
        assert n % 2 == 0
        return self.f32(n // 2).bitcast(BF16)


def build_program(debug=False, upto=9):
    nc = bass.Bass("TRN2", target_bir_lowering=False)
    okind = "ExternalOutput" if debug else "Internal"

    def din(name, shape, dt=F32):
        return nc.dram_tensor(name, list(shape), dt, kind="ExternalInput").ap()

    xtok = din("xtok", [NTOK, D])
    c2_d = din("c2", [128, 16])
    adaw_d = din("ada_w", [D, 6 * D])
    adab_d = din("ada_b", [1, 6 * D])
    n1g_d = din("n1g", [128, 8])
    n2g_d = din("n2g", [128, 8])
    win_d = din("w_in", [D, 3072])
    wout_d = din("w_out", [D, D])
    lam_d = din("lamv", [256])
    dng_d = din("dng", [128])
    rng_d = din("rng", [128])
    rdec_d = din("rdec", [8])
    rw_d = din("router_w", [D, NE])
    rb_d = din("router_b", [NE])
    ew1_d = din("e_w1", [(NE + 1) * 128, 2048])
    ew3_d = din("e_w3", [(NE + 1) * 128, 2048])
    ew2_d = din("e_w2", [(NE + 1) * 128, 2048])
    utri_d = din("utri", [128, 128], BF16)
    onesb_d = din("onesb", [128, 128], BF16)
    j512_d = din("j512", [128, 8])
    b512_d = din("b512", [128, 128])
    iotap_d = din("iotap", [128, 1])
    xbase_d = din("xbase", [128, 512])
    ioti_d = din("ioti", [128, 512])
    fng_d = din("fng", [D])
    ident_d = din("ident", [128, 128], BF16)
    ropeC_d = din("ropeC", [128, NTOK])
    ropeS_d = din("ropeS", [128, NTOK])
    idxe_d = din("idxe", [128, 512])
    diagf_d = din("diagf", [128, 2048])
    diagb_d = din("diagb", [128, 2048])
    iota_d = din("iota128", [128, 32])
    offo_d = din("offo", [128, NQB * 32])
    offcf_d = din("offcf", [128, NQB * 2])
    offcb_d = din("offcb", [128, NQB * 2])
    selv_d = din("selv", [128, 2])
    y_d = nc.dram_tensor("y", [HALF, D], F32, kind="ExternalOutput").ap()

    QT = nc.dram_tensor("QT", [768, HALF], BF16, kind=okind).ap()
    KT = nc.dram_tensor("KT", [768, NTOK], BF16, kind=okind).ap()
    VS = nc.dram_tensor("VS", [NTOK, 1024], BF16, kind=okind).ap()
    RGT = nc.dram_tensor("RGT", [512, HALF], F32, kind=okind).ap()
    CATT = nc.dram_tensor("CATT", [512, HALF], BF16, kind=okind).ap()
    CAT = nc.dram_tensor("CAT", [HALF, 1024], BF16, kind=okind).ap()
    X1 = nc.dram_tensor("X1", [HALF, D], F32, kind=okind).ap()
    XN = nc.dram_tensor("XN", [HALF, D], BF16, kind=okind).ap()
    EWB = [nc.dram_tensor("EWB%d" % i, [NE * 128, 2048], BF16, kind="Internal").ap() for i in range(3)]
    NSLOT = (HALF * 8 // 512 + NE) * 512
    XS = nc.dram_tensor("XS", [NSLOT, D], BF16, kind="Internal").ap()
    YS = nc.dram_tensor("YS", [NSLOT, D], F32, kind="Internal").ap()
    if debug:
        dbg_dk = nc.dram_tensor("dbg_dk", [128, 256], F32, kind="ExternalOutput").ap()
        dbg_wk = nc.dram_tensor("dbg_wk", [128, 256], F32, kind="ExternalOutput").ap()
        dbg_eb = nc.dram_tensor("dbg_eb", [128, 128], F32, kind="ExternalOutput").ap()

    with contextlib.ExitStack() as st:
        def sb(name, shape, dt=F32):
            return st.enter_context(nc.sbuf_tensor(name, list(shape), dt))

        NAR = 37000
        arena_t = sb("arena", [128, NAR])
        AR = Arena(arena_t, NAR)
        ident = sb("ident_sb", [128, 128], BF16)
        Ax = sb("Ax", [128, 8]); Bx = sb("Bx", [128, 8])
        Ac = sb("Ac", [128, 8]); Bc = sb("Bc", [128, 8])
        A2 = sb("A2", [128, 8]); B2 = sb("B2", [128, 8])
        g1bc = sb("g1bc", [128, D]); g2bc = sb("g2bc", [128, D]); fgbc = sb("fgbc", [128, D])
        neglam = sb("neglam", [128, 1])
        again = sb("again", [128, 128]); rgain = sb("rgain", [128, 128])
        lg = sb("lg", [128, 8]); nlg = sb("nlg", [128, 8])
        rbias = sb("rbias", [128, NE])
        stat = sb("stat", [128, 64])
        SELM = sb("SELM", [128, 32 * NE]); GWt = sb("GWt", [128, 32 * NE]); TOP8 = sb("TOP8", [128, 32 * 8])
        Mall = sb("Mall", [128, 32 * NE], BF16)
        cvf = [sb("cvf0", [128, 2048]), sb("cvf1", [128, 2048])]
        cvb = [sb("cvb0", [128, 2048], BF16), sb("cvb1", [128, 2048], BF16)]
        cvi = {"i": 0}

        def emit_preconv(e_lo, e_hi):
            for ex in range(e_lo, e_hi):
                for mi, tab in enumerate((ew1_d, ew3_d, ew2_d)):
                    i = cvi["i"] % 2
                    cvi["i"] += 1
                    P.dma(lambda e, tab=tab, ex=ex, i=i: e.dma_start(out=cvf[i][:], in_=tab[ex * 128:(ex + 1) * 128, :]),
                          writes=["cvf%d" % i], eng="pool")
                    if mi < 2:
                        P.op("pool", lambda e, i=i: e.tensor_copy(out=cvb[i][:], in_=cvf[i][:]), reads=["cvf%d" % i], writes=["cvb%d" % i])
                    else:
                        for k2 in range(2):
                            P.op("pool", lambda e, i=i, k2=k2: e.tensor_tensor(
                                out=cvb[i][:, k2 * 1024:(k2 + 1) * 1024], in0=cvf[i][:, k2 * 1024:(k2 + 1) * 1024], in1=g2bc[:], op=ALU.mult),
                                reads=["cvf%d" % i, "g2bc"], writes=["cvb%d" % i])
                    P.dma(lambda e, mi=mi, ex=ex, i=i: e.dma_start(out=EWB[mi][ex * 128:(ex + 1) * 128, :], in_=cvb[i][:]),
                          reads=["cvb%d" % i], writes=["EWB%d_%d" % (mi, ex)], eng="pool")
        psum_all = st.enter_context(nc.psum_tensor("psum_all", [128, 4096], F32))
        banks = [psum_all[:, i * 512:(i + 1) * 512] for i in range(8)]
        P = Prog(nc)

        def phase0():
            AR.reset()
            c2 = AR.f32(16)
            c2s = AR.f32(16)
            c2bc = AR.f32(8 * 128)
            ones_row = AR.f32(128)
            adab = AR.f32(6 * D)
            modT = AR.f32(96)
            adaw = [AR.f32(8 * 1024), AR.f32(8 * 1024)]
            n1g = AR.f32(8); n2g = AR.f32(8)
            lamt = AR.f32(256); lamp = AR.f32(128); lams = AR.f32(2)
            rdec = AR.f32(8)
            c2s3 = c2s.rearrange("p (k t) -> p k t", t=2)
            c2bc3 = c2bc.rearrange("p (k n) -> p k n", n=128)
            modT3 = modT.rearrange("p (j t) -> p j t", t=2)
            modps = banks[0][:, 0:96].rearrange("p (j t) -> p j t", t=2)

            P.dma(lambda e: e.dma_start(out=ident[:], in_=ident_d), writes=["ident"])
            P.dma(lambda e: e.dma_start(out=c2, in_=c2_d), writes=["c2"])
            P.dma(lambda e: e.dma_start(out=adab[0:1, :], in_=adab_d), writes=["adab"])
            P.dma(lambda e: e.dma_start(out=n1g, in_=n1g_d), writes=["n1g"])
            P.dma(lambda e: e.dma_start(out=n2g, in_=n2g_d), writes=["n2g"])
            P.dma(lambda e: e.dma_start(out=fgbc[:], in_=fng_d.partition_broadcast(128)), writes=["fgbc"])
            P.dma(lambda e: e.dma_start(out=lamt, in_=lam_d.partition_broadcast(128)), writes=["lamt"])
            P.dma(lambda e: e.dma_start(out=again[:], in_=dng_d.partition_broadcast(128)), writes=["again"])
            P.dma(lambda e: e.dma_start(out=rgain[:], in_=rng_d.partition_broadcast(128)), writes=["rgain"])
            P.dma(lambda e: e.dma_start(out=rdec, in_=rdec_d.partition_broadcast(128)), writes=["rdec"])
            P.dma(lambda e: e.dma_start(out=rbias[:], in_=rb_d.partition_broadcast(128)), writes=["rbias"])
            P.op("dve", lambda e: e.memset(ones_row, 1.0), writes=["ones"])
            P.op("act", lambda e: e.activation(out=c2s, in_=c2, func=AF.Silu), reads=["c2"], writes=["c2s"])
            P.op("dve", lambda e: e.tensor_copy(out=c2bc3, in_=c2s3[:, :, 0:1].to_broadcast([128, 8, 128])),
                 reads=["c2s"], writes=["c2bc"])
            P.op("dve", lambda e: e.tensor_scalar(out=again[:], in0=again[:], scalar1=0.8, scalar2=None, op0=ALU.mult),
                 reads=["again"], writes=["again"])
            lt = lamt.rearrange("p (a n) -> p a n", n=64)
            lp = lamp.rearrange("p (a n) -> p a n", n=64)
            P.op("dve", lambda e: e.tensor_tensor(out=lp, in0=lt[:, 0:2, :], in1=lt[:, 2:4, :], op=ALU.mult),
                 reads=["lamt"], writes=["lamp"])
            P.op("dve", lambda e: e.tensor_reduce(out=lams, in_=lp, axis=AX.X, op=ALU.add), reads=["lamp"], writes=["lams"])
            P.op("act", lambda e: e.activation(out=lams, in_=lams, func=AF.Exp), reads=["lams"], writes=["lams"])
            P.op("dve", lambda e: e.scalar_tensor_tensor(out=neglam[:], in0=lams[:, 1:2], scalar=-0.2, in1=lams[:, 0:1],
                                                          op0=ALU.add, op1=ALU.subtract),
                 reads=["lams"], writes=["neglam"])
            P.op("act", lambda e: e.activation(out=lg[:], in_=rdec, func=AF.Exp, scale=-1.0), reads=["rdec"], writes=["lg"])
            P.op("dve", lambda e: e.tensor_scalar(out=lg[:], in0=lg[:], scalar1=1.0, scalar2=None, op0=ALU.add),
                 reads=["lg"], writes=["lg"])
            P.op("act", lambda e: e.activation(out=nlg[:], in_=lg[:], func=AF.Ln), reads=["lg"], writes=["nlg"])
            P.op("dve", lambda e: e.tensor_scalar(out=lg[:], in0=nlg[:], scalar1=-1.0, scalar2=None, op0=ALU.mult),
                 reads=["nlg"], writes=["lg"])

            for q in range(6):
                aw = adaw[q % 2]
                aw3 = aw.rearrange("p (k c) -> p k c", c=1024)
                P.dma(lambda e, q=q, aw3=aw3: e.dma_start(
                    out=aw3, in_=adaw_d[:, q * 1024:(q + 1) * 1024].rearrange("(k p) c -> p k c", p=128)),
                    writes=["adaw%d" % (q % 2)])

                def mm(e, q=q, aw3=aw3):
                    for jj in range(8):
                        j = q * 8 + jj
                        for kc in range(8):
                            e.matmul(modps[:, j, :], lhsT=aw3[:, kc, jj * 128:(jj + 1) * 128], rhs=c2s3[:, kc, :],
                                     start=(kc == 0), stop=False)
                        ins = e.matmul(modps[:, j, :], lhsT=adab[0:1, j * 128:(j + 1) * 128], rhs=ones_row[0:1, 0:2],
                                       start=False, stop=True)
                    return ins
                P.op("pe", mm, reads=["adaw%d" % (q % 2), "c2s", "adab", "ones"], writes=["modps"])
                if q in (2, 5):
                    gdst = g1bc if q == 2 else g2bc

                    def mmb(e, q=q, aw3=aw3):
                        for hh in range(2):
                            for kc in range(8):
                                e.matmul(banks[1 + hh][:], lhsT=c2bc3[:, kc, :], rhs=aw3[:, kc, hh * 512:(hh + 1) * 512],
                                         start=(kc == 0), stop=False)
                            ins = e.matmul(banks[1 + hh][:], lhsT=ones_row[0:1, 0:128],
                                           rhs=adab[0:1, q * 1024 + hh * 512:q * 1024 + (hh + 1) * 512],
                                           start=False, stop=True)
                        return ins
                    P.op("pe", mmb, reads=["adaw%d" % (q % 2), "c2bc", "adab", "ones", "gdst%d" % q], writes=["gps"])
                    for hh in range(2):
                        P.op("dve", lambda e, hh=hh, gdst=gdst: e.tensor_copy(out=gdst[:, hh * 512:(hh + 1) * 512], in_=banks[1 + hh][:]),
                             reads=["gps"], writes=["gdst%d" % q])
            P.op("dve", lambda e: e.tensor_copy(out=modT, in_=banks[0][:, 0:96]), reads=["modps"], writes=["modT"])
            for (Adst, Bdst, jsc, jsh, col, gsrc, nm) in ((Ax, Bx, 8, 0, 0, n1g, "x"), (Ac, Bc, 8, 0, 1, n1g, "c"),
                                                          (A2, B2, 32, 24, 0, n2g, "2")):
                P.op("dve", lambda e, Adst=Adst, jsc=jsc, col=col, gsrc=gsrc: e.scalar_tensor_tensor(
                    out=Adst[:], in0=modT3[:, jsc:jsc + 8, col], scalar=1.0, in1=gsrc, op0=ALU.add, op1=ALU.mult),
                    reads=["modT", "n1g", "n2g"], writes=["A" + nm])
                P.op("dve", lambda e, Bdst=Bdst, jsh=jsh, col=col: e.tensor_copy(out=Bdst[:], in_=modT3[:, jsh:jsh + 8, col]),
                     reads=["modT"], writes=["B" + nm])
            P.barrier()

        def norm_block(nt, load_fn, xts, xns, junk, hT3, Acol, Bcol, tag, after_xn=None, hname=None):
            N = nt * 128
            PB = ["pb0", "pb1", "pb2", "pb3"]
            for t in range(nt):
                xt = xts[t % len(xts)]
                xn = xns[t % len(xns)]
                xr = "%s_x%d" % (tag, t % len(xts))
                nr = "%s_n%d" % (tag, t % len(xns))
                load_fn(t, xt, xr)
                ssq = stat[:, t:t + 1]
                rs = stat[:, 8 + t:9 + t]
                P.op("act", lambda e, xt=xt, ssq=ssq: e.activation(out=junk, in_=xt, func=AF.Square, accum_out=ssq),
                     reads=[xr], writes=["junk", "ss%d" % t])
                P.op("dve", lambda e, ssq=ssq, rs=rs: e.tensor_scalar(out=rs, in0=ssq, scalar1=1.0 / D, scalar2=EPS,
                                                                      op0=ALU.mult, op1=ALU.add),
                     reads=["ss%d" % t], writes=["rs%d" % t])
                P.op("act", lambda e, rs=rs: e.activation(out=rs, in_=rs, func=AF.Sqrt), reads=["rs%d" % t], writes=["rs%d" % t])
                P.op("dve", lambda e, rs=rs: e.reciprocal(out=rs, in_=rs), reads=["rs%d" % t], writes=["rs%d" % t])
                P.op("dve", lambda e, xt=xt, xn=xn, rs=rs: e.tensor_scalar(out=xn, in0=xt, scalar1=rs, scalar2=None, op0=ALU.mult),
                     reads=[xr, "rs%d" % t], writes=[nr])
                if after_xn is not None:
                    after_xn(t, xn, nr)

                def tr(e, xn=xn, t=t):
                    for kc in range(8):
                        pv = banks[kc // 2][:].bitcast(BF16)
                        ins = e.transpose(out=pv[:, (kc % 2) * 512 + t * 128:(kc % 2) * 512 + (t + 1) * 128],
                                          in_=xn[:, kc * 128:(kc + 1) * 128], identity=ident[:])
                    return ins
                P.op("pe", tr, reads=[nr, "ident"], writes=PB)
            for kc in range(8):
                pv = banks[kc // 2][:].bitcast(BF16)
                P.op("act", lambda e, kc=kc, pv=pv: e.activation(
                    out=hT3[:, kc, 0:N], in_=pv[:, (kc % 2) * 512:(kc % 2) * 512 + N], func=AF.Identity,
                    scale=Acol[:, kc:kc + 1], bias=Bcol[:, kc:kc + 1]),
                    reads=[PB[kc // 2]], writes=[hname or ("hT_" + tag)])

        def phase1():
            AR.reset()
            Wb = AR.bf16(8 * 3072)
            Wr = AR.bf16(8 * 1536)
            wst = [AR.f32(8 * 256), AR.f32(8 * 256)]
            xts = [AR.f32(1024), AR.f32(1024)]
            xns = [AR.bf16(1024), AR.bf16(1024)]
            junk = AR.bf16(1024)
            hTs = [AR.bf16(8 * 512), AR.bf16(8 * 512)]
            cosbs = [AR.f32(512), AR.f32(512)]; sinbs = [AR.f32(512), AR.f32(512)]
            t1s = [AR.f32(512), AR.f32(512)]; t2s = [AR.f32(512), AR.f32(512)]
            ofm = [AR.bf16(512), AR.bf16(512)]
            vt = [AR.bf16(1024), AR.bf16(1024)]
            rgt = [AR.f32(512), AR.f32(512)]
            Wb3 = Wb.rearrange("p (k c) -> p k c", c=3072)
            Wr3 = Wr.rearrange("p (k c) -> p k c", c=1536)
            hT3s = [h_.rearrange("p (k n) -> p k n", n=512) for h_ in hTs]
            for q in range(12):
                ws = wst[q % 2].rearrange("p (k c) -> p k c", c=256)
                P.dma(lambda e, q=q, ws=ws: e.dma_start(
                    out=ws, in_=win_d[:, q * 256:(q + 1) * 256].rearrange("(k p) c -> p k c", p=128)),
                    writes=["wst%d" % (q % 2)])
                P.op("dve", lambda e, q=q, ws=ws: e.tensor_copy(out=Wb3[:, :, q * 256:(q + 1) * 256], in_=ws),
                     reads=["wst%d" % (q % 2)], writes=["Wb"])
            for gi, (c0, ncol) in enumerate(((0, 1024), (1536, 512))):
                r0 = 0 if gi == 0 else 1024
                for kc in range(8):
                    src = Wb3[:, kc, c0:c0 + ncol].rearrange("p (g h f) -> p g h f", h=2, f=16)
                    dst = Wr3[:, kc, r0:r0 + ncol].rearrange("p (g h f) -> p g h f", h=2, f=16)
                    P.op("dve", lambda e, src=src, dst=dst: e.tensor_scalar(
                        out=dst[:, :, 0, :], in0=src[:, :, 1, :], scalar1=-1.0, scalar2=None, op0=ALU.mult),
                        reads=["Wb"], writes=["Wr"])
                    P.op("act", lambda e, src=src, dst=dst: e.activation(out=dst[:, :, 1, :], in_=src[:, :, 0, :], func=AF.Identity),
                         reads=["Wb"], writes=["Wr"])
            fm = []
            for i in range(4):
                fm.append((i * 128, i * 128, QT, i * 128, True))
            for i in range(4):
                fm.append((512 + i * 128, 512 + i * 128, KT, i * 128, False))
            for i in range(2):
                fm.append((1536 + i * 128, 1024 + i * 128, QT, 512 + i * 128, True))
            for i in range(2):
                fm.append((1792 + i * 128, 1280 + i * 128, KT, 512 + i * 128, False))
            blocks = [(0, 2, "ctx")] + [(CTX + i * 512, 4, "own") for i in range(8)] + \
                     [(CTX + HALF + i * 512, 4, "oth") for i in range(8)]
            st1 = {"cnt": 0, "tmc": 0, "tmb": 0}

            def do_norm(bi):
                tok0, nt, kind = blocks[bi]
                N = nt * 128
                cosb = cosbs[bi % 2]; sinb = sinbs[bi % 2]

                def load(t, xt, xr, tok0=tok0):
                    P.dma(lambda e: e.dma_start(out=xt, in_=xtok[tok0 + t * 128:tok0 + (t + 1) * 128, :]), writes=[xr])
                norm_block(nt, load, xts, xns, junk, hT3s[bi % 2], Ac if kind == "ctx" else Ax, Bc if kind == "ctx" else Bx, "p1",
                           hname="hT_p1_%d" % (bi % 2))
                P.dma(lambda e: e.dma_start(out=cosb[:, 0:N], in_=ropeC_d[:, tok0:tok0 + N]), writes=["cosb%d" % (bi % 2)])
                P.dma(lambda e: e.dma_start(out=sinb[:, 0:N], in_=ropeS_d[:, tok0:tok0 + N]), writes=["sinb%d" % (bi % 2)])

            def do_proj(bi, part):
                tok0, nt, kind = blocks[bi]
                N = nt * 128
                hT3 = hT3s[bi % 2]
                cosb = cosbs[bi % 2]; sinb = sinbs[bi % 2]
                hn = "hT_p1_%d" % (bi % 2); cn = "cosb%d" % (bi % 2); sn_ = "sinb%d" % (bi % 2)
                cnt = st1["cnt"]; tmc = st1["tmc"]; tmb = st1["tmb"]
                for (wc, rc, dst, drow, own_only) in (fm if part == "fm" else []):
                    if own_only and kind != "own":
                        continue

                    ba, bb = (4, 5) if cnt % 2 == 0 else (6, 7)
                    t1 = t1s[cnt % 2]; t2 = t2s[cnt % 2]
                    t1n = "t1_%d" % (cnt % 2); t2n = "t2_%d" % (cnt % 2)

                    def mm(e, wc=wc, rc=rc, N=N, ba=ba, bb=bb):
                        for kc in range(8):
                            e.matmul(banks[ba][:, 0:N], lhsT=Wb3[:, kc, wc:wc + 128], rhs=hT3[:, kc, 0:N],
                                     start=(kc == 0), stop=(kc == 7))
                        for kc in range(8):
                            ins = e.matmul(banks[bb][:, 0:N], lhsT=Wr3[:, kc, rc:rc + 128], rhs=hT3[:, kc, 0:N],
                                           start=(kc == 0), stop=(kc == 7))
                        return ins
                    P.op("pe", mm, reads=["Wb", "Wr", hn], writes=["ps%d" % ba, "ps%d" % bb])
                    P.op("dve", lambda e, N=N, ba=ba, t1=t1: e.tensor_tensor(out=t1[:, 0:N], in0=banks[ba][:, 0:N], in1=cosb[:, 0:N], op=ALU.mult),
                         reads=["ps%d" % ba, cn], writes=[t1n])
                    P.op("dve", lambda e, N=N, bb=bb, t2=t2: e.tensor_tensor(out=t2[:, 0:N], in0=banks[bb][:, 0:N], in1=sinb[:, 0:N], op=ALU.mult),
                         reads=["ps%d" % bb, sn_], writes=[t2n])
                    o = ofm[cnt % 2]
                    on = "ofm%d" % (cnt % 2)
                    cnt += 1
                    P.op("dve", lambda e, o=o, N=N, t1=t1, t2=t2: e.tensor_tensor(out=o[:, 0:N], in0=t1[:, 0:N], in1=t2[:, 0:N], op=ALU.add),
                         reads=[t1n, t2n], writes=[on])
                    if dst is QT:
                        c0 = tok0 - CTX
                    else:
                        c0 = tok0
                    P.dma(lambda e, o=o, dst=dst, drow=drow, c0=c0, N=N: e.dma_start(out=dst[drow:drow + 128, c0:c0 + N], in_=o[:, 0:N]),
                          reads=[on], eng="pool")
                for t in range(nt if part == "tm" else 0):
                    v = vt[tmc % 2]
                    vn = "vt%d" % (tmc % 2)
                    rgb = rgt[tmc % 2]
                    rn = "rgt%d" % (tmc % 2)
                    tmc += 1
                    groups = [(1024, 4 + (tmb % 4), v[:, 0:512], vn, "copy"), (2048, 4 + ((tmb + 1) % 4), v[:, 512:1024], vn, "copy")]
                    tmb += 2
                    for (wc, bk, dst_sb, dn, how) in groups:
                        def mmt(e, wc=wc, bk=bk, t=t):
                            for kc in range(8):
                                ins = e.matmul(banks[bk][:], lhsT=hT3[:, kc, t * 128:(t + 1) * 128], rhs=Wb3[:, kc, wc:wc + 512],
                                               start=(kc == 0), stop=(kc == 7))
                            return ins
                        P.op("pe", mmt, reads=["Wb", hn, "ev%d" % bk], writes=["ps%d" % bk])
                        if how == "copy" and wc == 1024:
                            P.op("dve", lambda e, bk=bk, dst_sb=dst_sb: e.tensor_copy(out=dst_sb, in_=banks[bk][:]),
                                 reads=["ps%d" % bk], writes=[dn, "ev%d" % bk])
                        elif how == "copy":
                            P.op("act", lambda e, bk=bk, dst_sb=dst_sb: e.activation(out=dst_sb, in_=banks[bk][:], func=AF.Identity),
                                 reads=["ps%d" % bk], writes=[dn, "ev%d" % bk])
                        else:
                            P.op("act", lambda e, bk=bk, dst_sb=dst_sb: e.activation(out=dst_sb, in_=banks[bk][:], func=AF.Silu),
                                 reads=["ps%d" % bk], writes=[dn, "ev%d" % bk])
                    r0 = tok0 + t * 128
                    P.dma(lambda e, v=v, r0=r0: e.dma_start(out=VS[r0:r0 + 128, :], in_=v), reads=[vn], eng="pool")
                if kind == "own" and part == "tm":
                    for i in range(4):
                        bk = 4 + (tmb % 4)
                        tmb += 1
                        rgb = rgt[tmc % 2]; rn = "rgt%d" % (tmc % 2)
                        tmc += 1

                        def mmg(e, i=i, bk=bk):
                            for kc in range(8):
                                ins = e.matmul(banks[bk][:], lhsT=Wb3[:, kc, 2560 + i * 128:2560 + (i + 1) * 128], rhs=hT3[:, kc, :],
                                               start=(kc == 0), stop=(kc == 7))
                            return ins
                        P.op("pe", mmg, reads=["Wb", hn], writes=["ps%d" % bk])
                        P.op("act", lambda e, bk=bk, rgb=rgb: e.activation(out=rgb, in_=banks[bk][:], func=AF.Silu),
                             reads=["ps%d" % bk], writes=[rn])
                        c0 = tok0 - CTX
                        P.dma(lambda e, rgb=rgb, i=i, c0=c0: e.dma_start(out=RGT[i * 128:(i + 1) * 128, c0:c0 + 512], in_=rgb), reads=[rn], eng="pool")
                st1["cnt"] = cnt; st1["tmc"] = tmc; st1["tmb"] = tmb

            do_norm(0)
            for bi in range(len(blocks)):
                do_proj(bi, "fm")
                if bi + 1 < len(blocks):
                    do_norm(bi + 1)
                do_proj(bi, "tm")
            P.barrier()

        def load_head_kv(KTh, Vh3, krow0, nrows, vcol0, tg):
            P.dma(lambda e: e.dma_start(out=KTh[0:nrows, :], in_=KT[krow0:krow0 + nrows, :]), writes=["KTh_%d" % tg])
            for g in range(6):
                P.dma(lambda e, g=g: e.dma_start(
                    out=Vh3[:, g * 11:(g + 1) * 11, 0:128],
                    in_=VS[g * 11 * 128:(g + 1) * 11 * 128, vcol0:vcol0 + 128].rearrange("(c p) e -> p c e", p=128)),
                    writes=["Vh%d_%d" % (g, tg)])

        def finalize_norm(o_sb, on, gain, extra_mul, out_bf, obn, idx, stat_junk):
            ssq = stat[:, 16 + idx:17 + idx]
            sn = "fs%d" % idx
            P.op("act", lambda e: e.activation(out=stat_junk, in_=o_sb, func=AF.Square, accum_out=ssq),
                 reads=[on], writes=["sjunk", sn])
            P.op("act", lambda e: e.activation(out=ssq, in_=ssq, func=AF.Ln, scale=1.0 / 128, bias=EPSc[:]), reads=[sn, "EPSc"], writes=[sn])
            P.op("act", lambda e: e.activation(out=ssq, in_=ssq, func=AF.Exp, scale=-0.5), reads=[sn], writes=[sn])
            if extra_mul is None:
                P.op("dve", lambda e: e.scalar_tensor_tensor(out=out_bf, in0=o_sb, scalar=ssq, in1=gain[:], op0=ALU.mult, op1=ALU.mult),
                     reads=[on, sn], writes=[obn])
            else:
                ex, exn = extra_mul
                P.op("dve", lambda e: e.scalar_tensor_tensor(out=o_sb, in0=o_sb, scalar=ssq, in1=gain[:], op0=ALU.mult, op1=ALU.mult),
                     reads=[on, sn], writes=[on])
                P.op("dve", lambda e: e.tensor_tensor(out=out_bf, in0=o_sb, in1=ex, op=ALU.mult),
                     reads=[on, exn], writes=[obn])

        def phase2_attention():
            AR.reset()
            emit_preconv(0, 52)
            KThs = [AR.bf16(NTOK), AR.bf16(NTOK)]
            Vh3s = [AR.bf16(NKC * 130).rearrange("p (c e) -> p c e", e=130) for _ in range(2)]
            QTb = [AR.bf16(512), AR.bf16(512)]
            NPT = 4
            PT = [AR.bf16(1024) for _ in range(NPT)]
            ocp = AR.f32(3 * 387)
            osb = [AR.f32(128), AR.f32(128)]
            obf = [AR.bf16(128), AR.bf16(128)]
            stat_junk = AR.bf16(128)
            for v_ in Vh3s:
                P.op("dve", lambda e, v_=v_: e.memset(v_[:, :, 128:129], 1.0), writes=["Vones"])
            zt = AR.bf16(8192)
            P.op("dve", lambda e: e.memset(zt, 0.0), writes=["zt"])
            zfill = list(range(NSLOT // 1024))

            def emit_zfill(n):
                for _ in range(n):
                    if zfill:
                        i = zfill.pop(0)
                        P.dma(lambda e, i=i: e.dma_start(out=XS[i * 1024:(i + 1) * 1024, :].rearrange("(p r) c -> p (r c)", r=8), in_=zt),
                              reads=["zt"], writes=["XSz%d" % i])
            oslot = {}
            lst = [(m, qs) for m in range(2) for qs in range(4)]
            for i, key in enumerate(lst):
                oslot[key] = (4 + i // 3, (i % 3) * 129)
            st_ = {"pti": 0, "sci": 0, "fin": 0, "kv": 0}

            def emit_qk(kc, qt, qn):
                pr_ = st_["sci"] % 2
                st_["sci"] += 1
                pt = PT[st_["pti"] % NPT]
                ptn = "PT%d" % (st_["pti"] % NPT)
                st_["pti"] += 1

                KTh = KThs[st_["kv"]]

                def qk(e, pr_=pr_, kc=kc, qt=qt, KTh=KTh):
                    for m in range(2):
                        ins = e.matmul(banks[pr_ * 2 + m][:], lhsT=KTh[m * 64:(m + 1) * 64, kc * 128:(kc + 1) * 128],
                                       rhs=qt[m * 64:(m + 1) * 64, :], start=True, stop=True)
                    return ins
                P.op("pe", qk, reads=["KTh_%d" % st_["kv"], qn], writes=["S%d" % pr_])
                P.op("act", lambda e, pr_=pr_, pt=pt: e.activation(out=pt, in_=psum_all[:, pr_ * 1024:(pr_ + 1) * 1024], func=AF.Exp, scale=0.125),
                     reads=["S%d" % pr_], writes=[ptn])
                return (pt, ptn)

            load_head_kv(KThs[0], Vh3s[0], 0, 128, 0, 0)
            for h in range(4):
                st_["kv"] = h % 2
                Vh3 = Vh3s[h % 2]
                vnames = ["Vh%d_%d" % (g_, h % 2) for g_ in range(6)]
                for qb in range(NQB):
                    if qb == 1 and h + 1 < 4:
                        load_head_kv(KThs[(h + 1) % 2], Vh3s[(h + 1) % 2], (h + 1) * 128, 128, (h + 1) * 128, (h + 1) % 2)
                    qt = QTb[qb % 2]
                    qn = "QTb%d" % (qb % 2)
                    P.dma(lambda e, qt=qt, h=h, qb=qb: e.dma_start(out=qt, in_=QT[h * 128:(h + 1) * 128, qb * 512:(qb + 1) * 512]),
                          writes=[qn])
                    emit_zfill(3)
                    pend = [emit_qk(0, qt, qn), emit_qk(1, qt, qn)]
                    for kc in range(NKC):
                        pts = pend.pop(0)
                        if kc + 2 < NKC:
                            pend.append(emit_qk(kc + 2, qt, qn))

                        def av(e, pts=pts, kc=kc, Vh3=Vh3):
                            for m in range(2):
                                for qs in range(4):
                                    bk, off = oslot[(m, qs)]
                                    ins = e.matmul(banks[bk][:, off:off + 129], lhsT=pts[0][:, m * 512 + qs * 128:m * 512 + (qs + 1) * 128],
                                                   rhs=Vh3[:, kc, 0:129], start=(kc == 0 and off == 0),
                                                   stop=(kc == NKC - 1 and (off == 258 or (bk == 6 and off == 129))))
                            return ins
                        P.op("pe", av, reads=[pts[1], "Vones"] + vnames, writes=["Oacc"])
                    for b3 in range(3):
                        ncol = 387 if b3 < 2 else 258
                        P.op("dve", lambda e, b3=b3, ncol=ncol: e.tensor_copy(out=ocp[:, b3 * 387:b3 * 387 + ncol], in_=banks[4 + b3][:, 0:ncol]),
                             reads=["Oacc"], writes=["ocp"])
                    for qs in range(4):
                        fi = st_["fin"] % 2
                        st_["fin"] += 1
                        o = osb[fi]; on = "osb%d" % fi
                        ob = obf[fi]; obn = "obf%d" % fi
                        b1, o1 = oslot[(0, qs)]
                        b2, o2 = oslot[(1, qs)]
                        c1 = (b1 - 4) * 387 + o1
                        c2 = (b2 - 4) * 387 + o2
                        r1 = stat[:, 24 + fi:25 + fi]
                        r2 = stat[:, 28 + fi:29 + fi]
                        rn = "r12_%d" % fi
                        P.op("dve", lambda e, c1=c1, r1=r1: e.reciprocal(out=r1, in_=ocp[:, c1 + 128:c1 + 129]),
                             reads=["ocp"], writes=[rn + "a"])
                        P.op("dve", lambda e, c2=c2, r2=r2: e.reciprocal(out=r2, in_=ocp[:, c2 + 128:c2 + 129]),
                             reads=["ocp"], writes=[rn + "b"])
                        P.op("dve", lambda e, r2=r2: e.tensor_tensor(out=r2, in0=r2, in1=neglam[:], op=ALU.mult),
                             reads=[rn + "b", "neglam"], writes=[rn + "b"])
                        P.op("dve", lambda e, o=o, c1=c1, r1=r1: e.tensor_scalar(
                            out=o, in0=ocp[:, c1:c1 + 128], scalar1=r1, scalar2=None, op0=ALU.mult),
                            reads=["ocp", rn + "a"], writes=[on])
                        P.op("dve", lambda e, o=o, c2=c2, r2=r2: e.scalar_tensor_tensor(
                            out=o, in0=ocp[:, c2:c2 + 128], scalar=r2, in1=o, op0=ALU.mult, op1=ALU.add),
                            reads=["ocp", rn + "b", on], writes=[on])
                        finalize_norm(o, on, again, None, ob, obn, fi, stat_junk)
                        row0 = qb * 512 + qs * 128
                        P.dma(lambda e, ob=ob, row0=row0, h=h: e.dma_start(out=CAT[row0:row0 + 128, h * 128:(h + 1) * 128], in_=ob),
                              reads=[obn])
            P.barrier()

        def phase3_retention():
            AR.reset()
            emit_preconv(52, NE)
            KThs = [AR.bf16(NTOK)]
            Vh3s = [AR.bf16(NKC * 130).rearrange("p (c e) -> p c e", e=130)]
            QTb = [AR.bf16(512), AR.bf16(512)]
            NAT = 8
            AT = [AR.bf16(512) for _ in range(NAT)]
            ocp = AR.f32(512); sq = AR.f32(512); rs = AR.f32(512); o2 = AR.f32(512)
            obf = [AR.bf16(512), AR.bf16(512)]
            gat = [AR.f32(512), AR.f32(512)]
            ones_f = AR.f32(128); rgcol = AR.f32(1)
            P.op("dve", lambda e: e.memset(ones_f, 1.0), writes=["ones_f"])
            P.dma(lambda e: e.dma_start(out=rgcol, in_=rng_d.rearrange("(p o) -> p o", o=1)), writes=["rgcol"])
            idxe = AR.f32(512)
            diagf = AR.f32(2048); diagb = AR.f32(2048)
            iot = AR.f32(32)
            offo = AR.f32(NQB * 32); offcf = AR.f32(NQB * 2); offcb = AR.f32(NQB * 2)
            selv = AR.f32(2)
            lgo = AR.f32(4); alo = AR.f32(4); tmp4 = AR.f32(4)
            Ef = AR.f32(512); Eb = AR.f32(512); Eo = AR.f32(512)
            Dd = AR.f32(2048); Dt = AR.f32(2048)
            CF = AR.f32(32); CB = AR.f32(32)
            Co = AR.f32(NQB * 32); Ccf = AR.f32(NQB * 2); Ccb = AR.f32(NQB * 2)
            Mc = [[AR.f32(512), AR.f32(512)], [AR.f32(512), AR.f32(512)]]
            ioti = AR.f32(512); jcol = AR.f32(1)
            XF = AR.f32(512); XB = AR.f32(512); XO = AR.f32(512)
            bF = AR.f32(1); bB = AR.f32(1); bO = AR.f32(1)
            SF = AR.f32(32); SB = AR.f32(32); SO = AR.f32(NQB * 32)
            qcp = [[AR.bf16(512) for _ in range(3)] for _ in range(2)]
            for k_ in KThs:
                P.op("dve", lambda e, k_=k_: e.memset(k_[64:128, :], 0.0), writes=["KTz"])
            for qb_ in range(2):
                P.op("dve", lambda e, qb_=qb_: e.memset(QTb[qb_][64:128, :], 0.0), writes=["QTz%d" % qb_])
                for c_ in range(3):
                    P.op("dve", lambda e, qb_=qb_, c_=c_: e.memset(qcp[qb_][c_][64:128, :], 0.0), writes=["qcz%d_%d" % (qb_, c_)])
            P.dma(lambda e: e.dma_start(out=ioti, in_=ioti_d), writes=["ioti"])
            P.dma(lambda e: e.dma_start(out=jcol, in_=iotap_d), writes=["jcol"])
            for (dst, src, nm) in ((idxe, idxe_d, "idxe"), (diagf, diagf_d, "diagf"), (diagb, diagb_d, "diagb"),
                                   (iot, iota_d, "iot"), (offo, offo_d, "offo"), (offcf, offcf_d, "offcf"),
                                   (offcb, offcb_d, "offcb"), (selv, selv_d, "selv")):
                P.dma(lambda e, dst=dst, src=src: e.dma_start(out=dst, in_=src), writes=[nm])
            P.op("dve", lambda e: e.tensor_scalar(out=lgo, in0=lg[:, 0:4], scalar1=selv[:, 0:1], scalar2=None, op0=ALU.mult),
                 reads=["selv", "lg"], writes=["lgo"])
            P.op("dve", lambda e: e.tensor_scalar(out=tmp4, in0=lg[:, 4:8], scalar1=selv[:, 1:2], scalar2=None, op0=ALU.mult),
                 reads=["selv", "lg"], writes=["tmp4"])
            P.op("dve", lambda e: e.tensor_tensor(out=alo, in0=lgo, in1=tmp4, op=ALU.subtract), reads=["lgo", "tmp4"], writes=["alo"])
            P.op("dve", lambda e: e.tensor_tensor(out=lgo, in0=lgo, in1=tmp4, op=ALU.add), reads=["lgo", "tmp4", "alo"], writes=["lgo"])
            st_ = {"ati": 0, "sci": 0, "fin": 0, "evi": 0, "kv": 0}

            def emit_qk_pair(j, qt, qn, qb):
                bks = [(0, 1), (2, 3), (6, 7)][st_["sci"] % 3]
                st_["sci"] += 1

                qsel = []
                for i2 in range(2):
                    kc = 2 * j + i2
                    ko = kc - 2
                    if kc < 2 or (kc < 34 and 4 * qb <= ko <= 4 * qb + 3):
                        qsel.append(qt)
                    elif kc < 34 and ko < 4 * qb:
                        qsel.append(qcp[qb % 2][0])
                    elif kc < 34:
                        qsel.append(qcp[qb % 2][1])
                    else:
                        qsel.append(qcp[qb % 2][2])

                KTh = KThs[st_["kv"]]

                def qk2(e, j=j, qsel=qsel, bks=bks, KTh=KTh):
                    for i2 in range(2):
                        kc = 2 * j + i2
                        ins = e.matmul(banks[bks[i2]][:], lhsT=KTh[:, kc * 128:(kc + 1) * 128], rhs=qsel[i2], start=True, stop=True)
                    return ins
                P.op("pe", qk2, reads=["KTh_%d" % st_["kv"], "KTz", qn, "qcp%d" % (qb % 2)], writes=["S%d" % bks[0], "S%d" % bks[1]])
                return [emit_mask(2 * j + i2, bks[i2], qb) for i2 in range(2)]

            def emit_mask(kc, bk, qb):
                at = AT[st_["ati"] % NAT]
                atn = "AT%d" % (st_["ati"] % NAT)
                st_["ati"] += 1
                ko = kc - 2
                sn = "S%d" % bk
                if kc < 2:
                    mc = Mc[qb % 2][kc]
                    P.op("dve", lambda e, bk=bk, at=at, mc=mc: e.tensor_tensor(out=at, in0=banks[bk][:], in1=mc, op=ALU.mult),
                         reads=[sn, "Mc%d" % (qb % 2)], writes=[atn])
                elif kc < 34 and 4 * qb <= ko <= 4 * qb + 3:
                    s_ = ko - 4 * qb
                    P.op("dve", lambda e, bk=bk, at=at, s_=s_: e.tensor_tensor(
                        out=at, in0=banks[bk][:], in1=Dd[:, s_ * 512:(s_ + 1) * 512], op=ALU.mult),
                        reads=[sn, "Dd"], writes=[atn])
                else:
                    if kc < 34 and ko < 4 * qb:
                        r = 4 * qb - ko
                        scl, scn = SF[:, r:r + 1], "SF"
                    elif kc < 34:
                        r = ko - 4 * qb
                        scl, scn = SB[:, r:r + 1], "SB"
                    else:
                        ci = qb * 32 + (kc - 34)
                        scl, scn = SO[:, ci:ci + 1], "SO"
                    if st_["evi"] % 5 < 3:
                        P.op("act", lambda e, bk=bk, at=at, scl=scl: e.activation(out=at, in_=banks[bk][:], func=AF.Identity, scale=scl),
                             reads=[sn, scn], writes=[atn])
                    else:
                        P.op("dve", lambda e, bk=bk, at=at, scl=scl: e.tensor_scalar(out=at, in0=banks[bk][:], scalar1=scl, scalar2=None, op0=ALU.mult),
                             reads=[sn, scn], writes=[atn])
                    st_["evi"] += 1
                return (at, atn)

            for h in range(4):
                lf = lg[:, h:h + 1]
                lb = lg[:, 4 + h:5 + h]
                nlb = nlg[:, 4 + h:5 + h]
                lo = lgo[:, h:h + 1]
                ao = alo[:, h:h + 1]
                P.op("act", lambda e, lf=lf: e.activation(out=Ef, in_=idxe, func=AF.Exp, scale=lf, bias=LN8c[:]),
                     reads=["idxe", "lg"], writes=["Ef"])
                P.op("act", lambda e, nlb=nlb: e.activation(out=Eb, in_=idxe, func=AF.Exp, scale=nlb, bias=LN8c[:]),
                     reads=["idxe", "lg"], writes=["Eb"])
                P.op("act", lambda e, lf=lf: e.activation(out=Dd, in_=diagf, func=AF.Exp, scale=lf, bias=LN8c[:]),
                     reads=["diagf", "lg"], writes=["Dd"])
                P.op("act", lambda e, lb=lb: e.activation(out=Dt, in_=diagb, func=AF.Exp, scale=lb, bias=LN8c[:]),
                     reads=["diagb", "lg"], writes=["Dt"])
                P.op("dve", lambda e: e.tensor_tensor(out=Dd, in0=Dd, in1=Dt, op=ALU.add), reads=["Dd", "Dt"], writes=["Dd"])
                nlf = nlg[:, h:h + 1]
                P.op("act", lambda e, lf=lf: e.activation(out=XF, in_=ioti, func=AF.Exp, scale=lf), reads=["ioti", "lg"], writes=["XF"])
                P.op("act", lambda e, nlb=nlb: e.activation(out=XB, in_=ioti, func=AF.Exp, scale=nlb), reads=["ioti", "lg"], writes=["XB"])
                P.op("act", lambda e, ao=ao: e.activation(out=XO, in_=ioti, func=AF.Exp, scale=ao), reads=["ioti", "alo"], writes=["XO"])
                P.op("dve", lambda e, nlf=nlf: e.tensor_scalar(out=bF, in0=jcol, scalar1=nlf, scalar2=LN8, op0=ALU.mult, op1=ALU.add),
                     reads=["jcol", "lg"], writes=["bF"])
                P.op("dve", lambda e, lb=lb: e.tensor_scalar(out=bB, in0=jcol, scalar1=lb, scalar2=LN8, op0=ALU.mult, op1=ALU.add),
                     reads=["jcol", "lg"], writes=["bB"])
                P.op("dve", lambda e, ao=ao: e.tensor_scalar(out=bO, in0=jcol, scalar1=ao, scalar2=-1.0, op0=ALU.mult, op1=ALU.mult),
                     reads=["jcol", "alo"], writes=["bO"])
                P.op("dve", lambda e: e.tensor_scalar(out=bO, in0=bO, scalar1=LN8, scalar2=None, op0=ALU.add), reads=["bO"], writes=["bO"])
                P.op("act", lambda e, lf=lf: e.activation(out=SF, in_=iot, func=AF.Exp, scale=lf, bias=bF[:, 0:1]), reads=["iot", "lg", "bF"], writes=["SF"])
                P.op("act", lambda e, lb=lb: e.activation(out=SB, in_=iot, func=AF.Exp, scale=lb, bias=bB[:, 0:1]), reads=["iot", "lg", "bB"], writes=["SB"])
                P.op("act", lambda e, lo=lo: e.activation(out=SO, in_=offo, func=AF.Exp, scale=lo, bias=bO[:, 0:1]), reads=["offo", "lgo", "bO"], writes=["SO"])
                P.op("act", lambda e, lf=lf: e.activation(out=Ccf, in_=offcf, func=AF.Exp, scale=lf), reads=["offcf", "lg"], writes=["Ccf"])
                P.op("act", lambda e, lb=lb: e.activation(out=Ccb, in_=offcb, func=AF.Exp, scale=lb), reads=["offcb", "lg"], writes=["Ccb"])
                load_head_kv(KThs[0], Vh3s[0], 512 + h * 64, 64, 512 + h * 128, 0)
                st_["kv"] = 0
                Vh3 = Vh3s[0]
                vnames = ["Vh%d_0" % g_ for g_ in range(6)]
                for qb in range(NQB):
                    qt = QTb[qb % 2]
                    qn = "QTb%d" % (qb % 2)
                    P.dma(lambda e, qt=qt, h=h, qb=qb: e.dma_start(
                        out=qt[0:64, :], in_=QT[512 + h * 64:512 + (h + 1) * 64, qb * 512:(qb + 1) * 512]), writes=[qn])
                    for ci_, (xt_, xn_) in enumerate(((XF, "XF"), (XB, "XB"), (XO, "XO"))):
                        P.op("dve", lambda e, qt=qt, ci_=ci_, xt_=xt_, qb=qb: e.tensor_tensor(
                            out=qcp[qb % 2][ci_][0:64, :], in0=qt[0:64, :], in1=xt_[0:64, :], op=ALU.mult),
                            reads=[qn, xn_], writes=["qcp%d" % (qb % 2)])
                    for cc in range(2):
                        mc = Mc[qb % 2][cc]
                        ci = qb * 2 + cc
                        P.op("dve", lambda e, mc=mc, ci=ci: e.tensor_scalar(out=mc, in0=Ef, scalar1=Ccf[:, ci:ci + 1], scalar2=None, op0=ALU.mult),
                             reads=["Ef", "Ccf"], writes=["Mc%d" % (qb % 2)])
                        P.op("dve", lambda e, mc=mc, ci=ci: e.scalar_tensor_tensor(
                            out=mc, in0=Eb, scalar=Ccb[:, ci:ci + 1], in1=mc, op0=ALU.mult, op1=ALU.add),
                            reads=["Eb", "Ccb", "Mc%d" % (qb % 2)], writes=["Mc%d" % (qb % 2)])
                    pend = [emit_qk_pair(0, qt, qn, qb), emit_qk_pair(1, qt, qn, qb)]
                    for j in range(NKC // 2):
                        cur = pend.pop(0)
                        if j + 2 < NKC // 2:
                            pend.append(emit_qk_pair(j + 2, qt, qn, qb))

                        def av2(e, cur=cur, j=j, Vh3=Vh3):
                            for i2 in range(2):
                                kc = 2 * j + i2
                                ins = e.matmul(banks[4][:], lhsT=Vh3[:, kc, 0:128], rhs=cur[i2][0],
                                               start=(kc == 0), stop=(kc == NKC - 1))
                            return ins
                        P.op("pe", av2, reads=[cur[0][1], cur[1][1]] + vnames, writes=["Oacc"])
                    fi = st_["fin"] % 2
                    st_["fin"] += 1
                    g = gat[fi]; gn = "gat%d" % fi
                    ob = obf[fi]; obn = "obf%d" % fi
                    P.dma(lambda e, g=g, h=h, qb=qb: e.dma_start(out=g, in_=RGT[h * 128:(h + 1) * 128, qb * 512:(qb + 1) * 512]), writes=[gn])
                    P.op("act", lambda e: e.activation(out=ocp, in_=banks[4][:], func=AF.Identity), reads=["Oacc"], writes=["ocp"])
                    P.op("act", lambda e: e.activation(out=sq, in_=ocp, func=AF.Square), reads=["ocp"], writes=["sq"])
                    P.op("pe", lambda e: e.matmul(banks[5][:], lhsT=ones_f, rhs=sq, start=True, stop=True), reads=["ones_f", "sq"], writes=["ssps"])
                    P.op("act", lambda e: e.activation(out=rs, in_=banks[5][:], func=AF.Ln, scale=1.0 / 128, bias=EPSc[:]),
                         reads=["ssps", "EPSc"], writes=["rs"])
                    P.op("act", lambda e: e.activation(out=rs, in_=rs, func=AF.Exp, scale=-0.5), reads=["rs"], writes=["rs"])
                    P.op("dve", lambda e: e.tensor_tensor(out=o2, in0=ocp, in1=rs, op=ALU.mult), reads=["ocp", "rs"], writes=["o2"])
                    P.op("dve", lambda e, ob=ob, g=g: e.scalar_tensor_tensor(out=ob, in0=o2, scalar=rgcol[:, 0:1], in1=g, op0=ALU.mult, op1=ALU.mult),
                         reads=["o2", "rgcol", gn], writes=[obn])
                    P.dma(lambda e, ob=ob, h=h, qb=qb: e.dma_start(out=CATT[h * 128:(h + 1) * 128, qb * 512:(qb + 1) * 512], in_=ob), reads=[obn])
            P.barrier()

        def phase4():
            AR.reset()
            Wo = AR.bf16(8 * 1024)
            wst = [AR.f32(8 * 512), AR.f32(8 * 512)]
            ct = [AR.bf16(512), AR.bf16(512)]
            cT = [AR.bf16(512), AR.bf16(512)]
            crT = [AR.bf16(4 * 512), AR.bf16(4 * 512)]
            xts = [AR.f32(1024) for _ in range(4)]
            xns = [AR.bf16(1024), AR.bf16(1024)]
            tmp = AR.f32(1024)
            junk = AR.bf16(1024)
            hT = AR.bf16(8 * 512)
            sw1 = AR.bf16(2048); sw3 = AR.bf16(2048); sw2 = AR.bf16(2048)
            sst = AR.f32(2048)
            rwst = AR.f32(8 * NE); rwb = AR.bf16(8 * NE)
            rt = AR.f32(512)
            sg = [AR.f32(512), AR.f32(512)]
            aT = AR.bf16(1024)
            Wo3 = Wo.rearrange("p (k c) -> p k c", c=1024)
            hT3 = hT.rearrange("p (k n) -> p k n", n=512)
            W1 = sw1.rearrange("p (k f) -> p k f", f=256)
            W3 = sw3.rearrange("p (k f) -> p k f", f=256)
            W2 = sw2.rearrange("p (k c) -> p k c", c=1024)
            a3 = aT.rearrange("p (f n) -> p f n", n=512)
            rwst3 = rwst.rearrange("p (k e) -> p k e", e=NE)
            rwb3 = rwb.rearrange("p (k e) -> p k e", e=NE)
            SELM3 = SELM[:].rearrange("p (t e) -> p t e", e=NE)
            GW3 = GWt[:].rearrange("p (t e) -> p t e", e=NE)
            TOP83 = TOP8[:].rearrange("p (t k) -> p t k", k=8)
            Mall3 = Mall[:].rearrange("p (t e) -> p t e", e=NE)
            for q in range(2):
                ws = wst[q].rearrange("p (k c) -> p k c", c=512)
                P.dma(lambda e, q=q, ws=ws: e.dma_start(
                    out=ws, in_=wout_d[:, q * 512:(q + 1) * 512].rearrange("(k p) c -> p k c", p=128)), writes=["wst%d" % q])
                P.op("dve", lambda e, q=q, ws=ws: e.tensor_copy(out=Wo3[:, :, q * 512:(q + 1) * 512], in_=ws),
                     reads=["wst%d" % q], writes=["Wo"])
            P.dma(lambda e: e.dma_start(out=rwst3, in_=rw_d.rearrange("(k p) e -> p k e", p=128)), writes=["rwst"])
            P.op("dve", lambda e: e.tensor_copy(out=rwb, in_=rwst), reads=["rwst"], writes=["rwb"])
            P.dma(lambda e: e.dma_start(out=sst, in_=ew1_d[NE * 128:(NE + 1) * 128, :]), writes=["sst"])
            P.op("dve", lambda e: e.tensor_copy(out=sw1, in_=sst), reads=["sst"], writes=["sw1"])
            P.dma(lambda e: e.dma_start(out=sst, in_=ew3_d[NE * 128:(NE + 1) * 128, :]), writes=["sst"])
            P.op("dve", lambda e: e.tensor_copy(out=sw3, in_=sst), reads=["sst"], writes=["sw3"])
            P.dma(lambda e: e.dma_start(out=sst, in_=ew2_d[NE * 128:(NE + 1) * 128, :]), writes=["sst"])
            for k2 in range(2):
                P.op("dve", lambda e, k2=k2: e.tensor_tensor(out=sw2[:, k2 * 1024:(k2 + 1) * 1024], in0=sst[:, k2 * 1024:(k2 + 1) * 1024],
                                                             in1=g2bc[:], op=ALU.mult), reads=["sst", "g2bc"], writes=["sw2"])
            sc = rt[:, 0:64]; sel = rt[:, 64:128]; eq = rt[:, 128:192]; sel2 = rt[:, 192:256]
            wts = rt[:, 320:384]
            m1 = rt[:, 384:392]; m2 = rt[:, 392:400]; gs = rt[:, 400:408]; top = rt[:, 408:416]
            gm = rt[:, 416:424]; wsum = rt[:, 432:433]
            v3 = lambda a: a.rearrange("p (g k) -> p g k", k=8)
            st_ = {"ci": 0, "sgi": 0}
            for blk in range(NQB):
                def load(t, xt, xr, blk=blk):
                    row0 = blk * 512 + t * 128
                    ci = st_["ci"]
                    st_["ci"] += 1
                    c = ct[ci % 2]; cn = "ct%d" % (ci % 2)
                    cTt = cT[ci % 2]; cTn = "cT%d" % (ci % 2)
                    P.dma(lambda e: e.dma_start(out=c, in_=CAT[row0:row0 + 128, 0:512]), writes=[cn])
                    cr3 = crT[blk % 2].rearrange("p (j n) -> p j n", n=512)
                    crn = "crT%d" % (blk % 2)
                    if t == 0:
                        P.dma(lambda e: e.dma_start(out=cr3, in_=CATT[:, blk * 512:(blk + 1) * 512].rearrange("(j p) n -> p j n", p=128)),
                              writes=[crn])
                    P.dma(lambda e: e.dma_start(out=xt, in_=xtok[CTX + row0:CTX + row0 + 128, :]), writes=[xr])

                    def tr(e):
                        pv = banks[4][:].bitcast(BF16)
                        for kc in range(4):
                            ins = e.transpose(out=pv[:, kc * 128:(kc + 1) * 128], in_=c[:, kc * 128:(kc + 1) * 128], identity=ident[:])
                        return ins
                    P.op("pe", tr, reads=[cn, "ident"], writes=["b4"])
                    P.op("act", lambda e: e.activation(out=cTt, in_=banks[4][:].bitcast(BF16)[:, 0:512], func=AF.Identity),
                         reads=["b4"], writes=[cTn])

                    def mm(e):
                        for hh in range(2):
                            for kc in range(8):
                                lt = cTt[:, kc * 128:(kc + 1) * 128] if kc < 4 else cr3[:, kc - 4, t * 128:(t + 1) * 128]
                                ins = e.matmul(banks[5 + hh][:], lhsT=lt,
                                               rhs=Wo3[:, kc, hh * 512:(hh + 1) * 512], start=(kc == 0), stop=(kc == 7))
                        return ins
                    P.op("pe", mm, reads=[cTn, crn, "Wo"], writes=["y5", "y6"])
                    for hh in range(2):
                        P.op("dve", lambda e, hh=hh: e.tensor_tensor(out=tmp[:, hh * 512:(hh + 1) * 512], in0=banks[5 + hh][:],
                                                                     in1=g1bc[:, hh * 512:(hh + 1) * 512], op=ALU.mult),
                             reads=["y%d" % (5 + hh), "g1bc"], writes=["tmp%d" % hh])
                    P.op("dve", lambda e: e.tensor_tensor(out=xt, in0=xt, in1=tmp, op=ALU.add), reads=[xr, "tmp0", "tmp1"], writes=[xr])

                def store_xn(t, xn, nr, blk=blk):
                    row0 = blk * 512 + t * 128
                    P.dma(lambda e: e.dma_start(out=XN[row0:row0 + 128, :], in_=xn), reads=[nr], eng="pool")
                norm_block(4, load, xts, xns, junk, hT3, A2, B2, "p4", after_xn=store_xn)
                for t in range(4):
                    tt = blk * 4 + t
                    selm = SELM3[:, tt, :]
                    top8 = TOP83[:, tt, :]

                    def rmm(e, t=t):
                        for kc in range(8):
                            ins = e.matmul(banks[7][:, 0:NE], lhsT=hT3[:, kc, t * 128:(t + 1) * 128], rhs=rwb3[:, kc, :],
                                           start=(kc == 0), stop=(kc == 7))
                        return ins
                    P.op("pe", rmm, reads=["hT_p4", "rwb"], writes=["y7"])
                    P.op("act", lambda e: e.activation(out=sc, in_=banks[7][:, 0:NE], func=AF.Sigmoid), reads=["y7"], writes=["sc"])
                    P.op("dve", lambda e: e.tensor_tensor(out=sel, in0=sc, in1=rbias[:], op=ALU.add), reads=["sc", "rbias"], writes=["sel"])
                    P.op("dve", lambda e: e.tensor_reduce(out=m1, in_=v3(sel), axis=AX.X, op=ALU.max), reads=["sel"], writes=["m1"])
                    P.op("dve", lambda e: e.tensor_tensor(out=v3(eq), in0=v3(sel), in1=m1.unsqueeze(2).to_broadcast([128, 8, 8]),
                                                          op=ALU.is_equal), reads=["sel", "m1"], writes=["eq"])
                    P.op("dve", lambda e: e.scalar_tensor_tensor(out=sel2, in0=eq, scalar=-1e30, in1=sel, op0=ALU.mult, op1=ALU.add),
                         reads=["eq", "sel"], writes=["sel2"])
                    P.op("dve", lambda e: e.tensor_reduce(out=m2, in_=v3(sel2), axis=AX.X, op=ALU.max), reads=["sel2"], writes=["m2"])
                    P.op("dve", lambda e: e.tensor_tensor(out=gs, in0=m1, in1=m2, op=ALU.add), reads=["m1", "m2"], writes=["gs"])
                    P.op("dve", lambda e: e.max(out=top, in_=gs), reads=["gs"], writes=["top"])
                    P.op("dve", lambda e: e.tensor_scalar(out=gm, in0=gs, scalar1=top[:, 3:4], scalar2=None, op0=ALU.is_ge),
                         reads=["gs", "top"], writes=["gm"])
                    P.op("dve", lambda e: e.tensor_scalar(out=gm, in0=gm, scalar1=-1.0, scalar2=1e30, op0=ALU.add, op1=ALU.mult),
                         reads=["gm"], writes=["gm"])
                    P.op("dve", lambda e, selm=selm: e.tensor_tensor(out=v3(selm), in0=v3(sel), in1=gm.unsqueeze(2).to_broadcast([128, 8, 8]),
                                                                     op=ALU.add), reads=["sel", "gm"], writes=["selm%d" % tt])
                    P.op("dve", lambda e, selm=selm, top8=top8: e.max(out=top8, in_=selm), reads=["selm%d" % tt], writes=["top8_%d" % tt])
                    P.op("dve", lambda e, selm=selm, top8=top8: e.tensor_scalar(out=eq, in0=selm, scalar1=top8[:, 7:8], scalar2=None, op0=ALU.is_ge),
                         reads=["selm%d" % tt, "top8_%d" % tt], writes=["eq"])
                    P.op("dve", lambda e, tt=tt: e.tensor_copy(out=Mall3[:, tt, :], in_=eq), reads=["eq"], writes=["Mall"])
                    P.op("dve", lambda e: e.tensor_tensor(out=wts, in0=sc, in1=eq, op=ALU.mult), reads=["sc", "eq"], writes=["wts"])
                    P.op("dve", lambda e: e.tensor_reduce(out=wsum, in_=wts, axis=AX.X, op=ALU.add), reads=["wts"], writes=["wsum"])
                    P.op("dve", lambda e: e.reciprocal(out=wsum, in_=wsum), reads=["wsum"], writes=["wsum"])
                    P.op("dve", lambda e, tt=tt: e.tensor_scalar(out=GW3[:, tt, :], in0=wts, scalar1=wsum, scalar2=2.5,
                                                                 op0=ALU.mult, op1=ALU.mult), reads=["wts", "wsum"], writes=["GW"])
                for fc in range(2):
                    s_ = sg[st_["sgi"] % 2]; sn = "sg%d" % (st_["sgi"] % 2)
                    st_["sgi"] += 1

                    def mm13(e, fc=fc):
                        for kc in range(8):
                            e.matmul(banks[0 + fc][:], lhsT=W1[:, kc, fc * 128:(fc + 1) * 128], rhs=hT3[:, kc, :],
                                     start=(kc == 0), stop=(kc == 7))
                        for kc in range(8):
                            ins = e.matmul(banks[2 + fc][:], lhsT=W3[:, kc, fc * 128:(fc + 1) * 128], rhs=hT3[:, kc, :],
                                           start=(kc == 0), stop=(kc == 7))
                        return ins
                    P.op("pe", mm13, reads=["sw1", "sw3", "hT_p4"], writes=["pb%d" % fc, "pb%d" % (2 + fc)])
                    P.op("act", lambda e, fc=fc, s_=s_: e.activation(out=s_, in_=banks[0 + fc][:], func=AF.Silu),
                         reads=["pb%d" % fc], writes=[sn])
                    P.op("dve", lambda e, fc=fc, s_=s_: e.tensor_tensor(out=a3[:, fc, :], in0=banks[2 + fc][:], in1=s_, op=ALU.mult),
                         reads=["pb%d" % (2 + fc), sn], writes=["aT"])
                for t in range(4):
                    xt = xts[t]
                    xr = "p4_x%d" % t
                    for hh in range(2):
                        bk = 5 + hh

                        def mm2(e, t=t, hh=hh, bk=bk):
                            for fc in range(2):
                                ins = e.matmul(banks[bk][:], lhsT=a3[:, fc, t * 128:(t + 1) * 128],
                                               rhs=W2[:, fc, hh * 512:(hh + 1) * 512], start=(fc == 0), stop=(fc == 1))
                            return ins
                        P.op("pe", mm2, reads=["aT", "sw2"], writes=["y%d" % bk])
                        xv = xt[:, hh * 512:(hh + 1) * 512]
                        P.op("dve", lambda e, bk=bk, xv=xv: e.tensor_tensor(out=xv, in0=banks[bk][:], in1=xv, op=ALU.add),
                             reads=["y%d" % bk, xr], writes=[xr])
                    row0 = blk * 512 + t * 128
                    P.dma(lambda e, xt=xt, row0=row0: e.dma_start(out=X1[row0:row0 + 128, :], in_=xt), reads=[xr], eng="pool")
            P.barrier()

        def phase5():
            I32 = mybir.dt.int32
            NBLK = HALF * 8 // 512 + NE
            NT = HALF // 128
            AR.reset()
            Utri = AR.bf16(128); ones_b = AR.bf16(128)
            J512 = AR.f32(8); B512 = AR.f32(128); iotap = AR.f32(1)
            CNT = AR.f32(64); NBk = AR.f32(64); PADD = AR.f32(64); ca = AR.f32(64); cb = AR.f32(64); PST = AR.f32(64)
            RANK = AR.f32(NT * 64)
            EB = AR.f32(NBLK); WIDXf = AR.f32(NBLK); WIDX = AR.f32(NBLK).bitcast(I32)
            DKf = AR.f32(NT * 8); DKi = AR.f32(NT * 8).bitcast(I32); WK = AR.f32(NT * 8)
            UNU = AR.f32(NBLK); XBASE = AR.f32(NBLK * 4); XIDXf = AR.f32(NBLK * 4); XIDX = AR.f32(NBLK * 4).bitcast(I32)
            regs = {}

            def breg(e, name, val):
                if name not in regs:
                    regs[name] = e.alloc_register(name)
                    e.reg_mov(regs[name], val)
                return regs[name]
            mark = AR.off
            cmp3 = AR.f32(NBLK * 64)
            oh = AR.f32(512); pr = AR.f32(512)
            SELM3 = SELM[:].rearrange("p (t e) -> p t e", e=NE)
            GW3 = GWt[:].rearrange("p (t e) -> p t e", e=NE)
            TOP83 = TOP8[:].rearrange("p (t k) -> p t k", k=8)
            Mall3 = Mall[:].rearrange("p (t e) -> p t e", e=NE)
            RANK3 = RANK.rearrange("p (t e) -> p t e", e=64)
            DKf3 = DKf.rearrange("p (t k) -> p t k", k=8)
            DKi3 = DKi.rearrange("p (t k) -> p t k", k=8)
            WK3 = WK.rearrange("p (t k) -> p t k", k=8)
            for (dst, src, nm) in ((Utri, utri_d, "Utri"), (ones_b, onesb_d, "ones_b"), (J512, j512_d, "J512"),
                                   (B512, b512_d, "B512"), (iotap, iotap_d, "iotap"), (XBASE, xbase_d, "XBASE")):
                P.dma(lambda e, dst=dst, src=src: e.dma_start(out=dst, in_=src), writes=[nm])
            def cnt_mm(e):
                for t in range(NT):
                    ins = e.matmul(banks[0][:, 0:64], lhsT=ones_b, rhs=Mall3[:, t, :], start=(t == 0), stop=(t == NT - 1))
                return ins
            P.op("pe", cnt_mm, reads=["ones_b", "Mall"], writes=["cntps"])
            P.op("dve", lambda e: e.tensor_copy(out=CNT, in_=banks[0][:, 0:64]), reads=["cntps"], writes=["CNT"])

            def rk_mm(e):
                for t in range(NT):
                    dst = banks[1 + t // 8][:, (t % 8) * 64:(t % 8 + 1) * 64]
                    ins = e.matmul(dst, lhsT=Utri, rhs=Mall3[:, t, :], start=True, stop=(t == 0))
                    for t2 in range(t):
                        ins = e.matmul(dst, lhsT=ones_b, rhs=Mall3[:, t2, :], start=False, stop=(t2 == t - 1))
                return ins
            P.op("pe", rk_mm, reads=["ones_b", "Utri", "Mall"], writes=["rkps"])
            for b4 in range(4):
                P.op("dve", lambda e, b4=b4: e.tensor_copy(out=RANK[:, b4 * 512:(b4 + 1) * 512], in_=banks[1 + b4][:]),
                     reads=["rkps"], writes=["RANK"])
            t8 = cmp3[:, 0:512].rearrange("p (e j) -> p e j", j=8)
            P.op("dve", lambda e: e.tensor_tensor(out=t8, in0=CNT.unsqueeze(2).to_broadcast([128, 64, 8]),
                                                  in1=J512.unsqueeze(1).to_broadcast([128, 64, 8]), op=ALU.is_gt),
                 reads=["CNT", "J512"], writes=["t8"])
            P.op("dve", lambda e: e.tensor_reduce(out=NBk, in_=t8, axis=AX.X, op=ALU.add), reads=["t8"], writes=["NBk"])
            P.op("dve", lambda e: e.tensor_scalar(out=PADD, in0=NBk, scalar1=512.0, scalar2=None, op0=ALU.mult), reads=["NBk"], writes=["PADD"])
            P.op("dve", lambda e: e.tensor_copy(out=ca, in_=PADD), reads=["PADD"], writes=["ca"])
            cur, nxt, cn_, nn_ = ca, cb, "ca", "cb"
            for sft in (1, 2, 4, 8, 16, 32):
                P.op("dve", lambda e, cur=cur, nxt=nxt, sft=sft: e.tensor_copy(out=nxt[:, 0:sft], in_=cur[:, 0:sft]), reads=[cn_], writes=[nn_])
                P.op("dve", lambda e, cur=cur, nxt=nxt, sft=sft: e.tensor_tensor(out=nxt[:, sft:64], in0=cur[:, sft:64], in1=cur[:, 0:64 - sft], op=ALU.add),
                     reads=[cn_, nn_], writes=[nn_])
                cur, nxt, cn_, nn_ = nxt, cur, nn_, cn_
            PEND, pend_n = cur, cn_
            P.op("dve", lambda e: e.tensor_tensor(out=PST, in0=PEND, in1=PADD, op=ALU.subtract), reads=[pend_n, "PADD"], writes=["PST"])
            P.op("dve", lambda e: e.tensor_tensor(out=RANK3, in0=RANK3, in1=PST.unsqueeze(1).to_broadcast([128, NT, 64]), op=ALU.add),
                 reads=["RANK", "PST"], writes=["RANK"])
            c3 = cmp3.rearrange("p (b e) -> p b e", e=64)
            P.op("dve", lambda e: e.tensor_tensor(out=c3, in0=PEND.unsqueeze(1).to_broadcast([128, NBLK, 64]),
                                                  in1=B512.unsqueeze(2).to_broadcast([128, NBLK, 64]), op=ALU.is_le),
                 reads=[pend_n, "B512", "t8"], writes=["c3"])
            P.op("dve", lambda e: e.tensor_reduce(out=EB, in_=c3, axis=AX.X, op=ALU.add), reads=["c3"], writes=["EB"])
            P.op("dve", lambda e: e.tensor_scalar(out=WIDXf, in0=EB, scalar1=128.0, scalar2=iotap[:, 0:1], op0=ALU.mult, op1=ALU.add),
                 reads=["EB", "iotap"], writes=["WIDXf"])
            P.op("dve", lambda e: e.tensor_copy(out=WIDX, in_=WIDXf), reads=["WIDXf"], writes=["WIDX"])
            P.op("dve", lambda e: e.tensor_scalar(out=UNU, in0=EB, scalar1=float(NE), scalar2=1.0e6, op0=ALU.is_ge, op1=ALU.mult),
                 reads=["EB"], writes=["UNU"])
            P.op("dve", lambda e: e.tensor_tensor(out=XIDXf.rearrange("p (b s) -> p b s", s=4), in0=XBASE.rearrange("p (b s) -> p b s", s=4),
                                                  in1=UNU.unsqueeze(2).to_broadcast([128, NBLK, 4]), op=ALU.add),
                 reads=["UNU", "XBASE"], writes=["XIDXf"])
            P.op("dve", lambda e: e.tensor_copy(out=XIDX, in_=XIDXf), reads=["XIDXf"], writes=["XIDX"])
            oh3 = oh.rearrange("p (k e) -> p k e", e=64)
            pr3 = pr.rearrange("p (k e) -> p k e", e=64)
            for t in range(NT):
                P.op("dve", lambda e, t=t: e.tensor_tensor(out=oh3, in0=SELM3[:, t, :].unsqueeze(1).to_broadcast([128, 8, 64]),
                                                           in1=TOP83[:, t, :].unsqueeze(2).to_broadcast([128, 8, 64]), op=ALU.is_equal),
                     reads=["c3"], writes=["oh"])
                P.op("dve", lambda e, t=t: e.tensor_tensor(out=pr3, in0=oh3, in1=RANK3[:, t, :].unsqueeze(1).to_broadcast([128, 8, 64]), op=ALU.mult),
                     reads=["oh", "RANK"], writes=["pr"])
                P.op("dve", lambda e, t=t: e.tensor_reduce(out=DKf3[:, t, :], in_=pr3, axis=AX.X, op=ALU.add), reads=["pr"], writes=["DKf"])
                P.op("dve", lambda e, t=t: e.tensor_tensor(out=pr3, in0=oh3, in1=GW3[:, t, :].unsqueeze(1).to_broadcast([128, 8, 64]), op=ALU.mult),
                     reads=["oh", "DKf"], writes=["pr"])
                P.op("dve", lambda e, t=t: e.tensor_reduce(out=WK3[:, t, :], in_=pr3, axis=AX.X, op=ALU.add), reads=["pr"], writes=["WK"])
            P.op("dve", lambda e: e.tensor_scalar(out=DKf, in0=DKf, scalar1=float(NBLK * 512 - 1), scalar2=0.0, op0=ALU.min, op1=ALU.max),
                 reads=["DKf"], writes=["DKf"])
            P.op("dve", lambda e: e.tensor_copy(out=DKi, in_=DKf), reads=["DKf"], writes=["DKi"])
            if debug:
                P.dma(lambda e: e.dma_start(out=dbg_dk, in_=DKf), reads=["DKf"])
                P.dma(lambda e: e.dma_start(out=dbg_wk, in_=WK), reads=["WK"])
                P.dma(lambda e: e.dma_start(out=dbg_eb, in_=EB), reads=["EB"])
            P.barrier()
            AR.off = mark
            xin = [AR.bf16(1024) for _ in range(4)]
            for t in range(NT):
                xi = xin[t % 4]; xn_ = "xin%d" % (t % 4)
                P.dma(lambda e, xi=xi, t=t: e.dma_start(out=xi, in_=XN[t * 128:(t + 1) * 128, :]), writes=[xn_])
                for k in range(8):
                    P.dma(lambda e, xi=xi, t=t, k=k: e.indirect_dma_start(
                        out=XS[:, :], out_offset=bass.IndirectOffsetOnAxis(ap=DKi3[:, t, k:k + 1], axis=0),
                        in_=xi, in_offset=None),
                        reads=[xn_, "DKi", "XSzero"], writes=["XS_%d_%d" % (t, k)], eng="pool")
            P.barrier()
            AR.off = mark
            w1b = [AR.bf16(2048), AR.bf16(2048)]; w3b = [AR.bf16(2048), AR.bf16(2048)]; w2b = [AR.bf16(2048), AR.bf16(2048)]
            xsb = [AR.bf16(4096), AR.bf16(4096)]
            xsTb = [AR.bf16(8 * 512), AR.bf16(8 * 512)]
            sg = [AR.f32(512), AR.f32(512)]
            aT = AR.bf16(1024)
            ysb = [AR.f32(4096), AR.f32(4096)]
            a3 = aT.rearrange("p (f n) -> p f n", n=512)
            st_ = {"sgi": 0, "evi": 0}

            def weights(b):
                wb = b % 2
                for (dst_, tab, nm) in ((w1b[wb], EWB[0], "w1b%d" % wb), (w3b[wb], EWB[1], "w3b%d" % wb), (w2b[wb], EWB[2], "w2b%d" % wb)):
                    P.dma(lambda e, dst_=dst_, tab=tab, b=b: e.indirect_dma_start(
                        out=dst_, out_offset=None, in_=tab[:, :],
                        in_offset=bass.IndirectOffsetOnAxis(ap=WIDX[:, b:b + 1], axis=0),
                        bounds_check=breg(e, "bcw", NE * 128 - 1), oob_is_err=False),
                        reads=["WIDX"], writes=[nm], eng="pool")

            def casts(b):
                pass

            def xs_load(b):
                xs3 = xsb[b % 2].rearrange("p (s c) -> p s c", c=1024)
                P.dma(lambda e, xs3=xs3, b=b: e.dma_start(out=xs3, in_=XS[b * 512:(b + 1) * 512, :].rearrange("(s p) c -> p s c", p=128)),
                      reads=["XS"], writes=["xsb%d_%d" % (b % 2, i4) for i4 in range(4)])

            def prep_half(b, half):
                xs3 = xsb[b % 2].rearrange("p (s c) -> p s c", c=1024)
                xsT3 = xsTb[b % 2].rearrange("p (k n) -> p k n", n=512)
                xtn = "xsT%d" % (b % 2)

                def tr(e, xs3=xs3, half=half):
                    for s4 in range(4):
                        for kk in range(4):
                            kc = half * 4 + kk
                            pv = banks[kk // 2][:].bitcast(BF16)
                            ins = e.transpose(out=pv[:, (kk % 2) * 512 + s4 * 128:(kk % 2) * 512 + (s4 + 1) * 128],
                                              in_=xs3[:, s4, kc * 128:(kc + 1) * 128], identity=ident[:])
                    return ins
                P.op("pe", tr, reads=["xsb%d_%d" % (b % 2, i4) for i4 in range(4)] + ["ident"], writes=["pb0", "pb1"])
                for kk in range(4):
                    kc = half * 4 + kk
                    pv = banks[kk // 2][:].bitcast(BF16)
                    src = pv[:, (kk % 2) * 512:(kk % 2) * 512 + 512]
                    if st_["evi"] % 2 == 0:
                        P.op("act", lambda e, kc=kc, src=src, xsT3=xsT3: e.activation(
                            out=xsT3[:, kc, :], in_=src, func=AF.Identity, scale=A2[:, kc:kc + 1], bias=B2[:, kc:kc + 1]),
                            reads=["pb%d" % (kk // 2)], writes=[xtn])
                    else:
                        P.op("dve", lambda e, kc=kc, src=src, xsT3=xsT3: e.tensor_scalar(
                            out=xsT3[:, kc, :], in0=src, scalar1=A2[:, kc:kc + 1], scalar2=B2[:, kc:kc + 1], op0=ALU.mult, op1=ALU.add),
                            reads=["pb%d" % (kk // 2)], writes=[xtn])
                    st_["evi"] += 1

            def up_gate(b, fc):
                wb = b % 2
                W1 = w1b[wb].rearrange("p (k f) -> p k f", f=256)
                W3 = w3b[wb].rearrange("p (k f) -> p k f", f=256)
                xsT3 = xsTb[b % 2].rearrange("p (k n) -> p k n", n=512)
                bu, bg = (2, 3) if fc == 0 else (4, 5)
                s_ = sg[st_["sgi"] % 2]; sn = "sg%d" % (st_["sgi"] % 2)
                st_["sgi"] += 1

                def mm13(e):
                    for kc in range(8):
                        e.matmul(banks[bu][:], lhsT=W1[:, kc, fc * 128:(fc + 1) * 128], rhs=xsT3[:, kc, :], start=(kc == 0), stop=(kc == 7))
                    for kc in range(8):
                        ins = e.matmul(banks[bg][:], lhsT=W3[:, kc, fc * 128:(fc + 1) * 128], rhs=xsT3[:, kc, :], start=(kc == 0), stop=(kc == 7))
                    return ins
                P.op("pe", mm13, reads=["w1b%d" % wb, "w3b%d" % wb, "xsT%d" % (b % 2)], writes=["pb%d" % bu, "pb%d" % bg])
                P.op("act", lambda e: e.activation(out=s_, in_=banks[bu][:], func=AF.Silu), reads=["pb%d" % bu], writes=[sn])
                P.op("dve", lambda e: e.tensor_tensor(out=a3[:, fc, :], in0=banks[bg][:], in1=s_, op=ALU.mult),
                     reads=["pb%d" % bg, sn], writes=["aT"])

            def down(b):
                wb = b % 2
                W2 = w2b[wb].rearrange("p (k c) -> p k c", c=1024)
                ys_ = ysb[b % 2]; ysn = "ysb%d" % (b % 2)
                ys3 = ys_.rearrange("p (s c) -> p s c", c=1024)
                ybanks = [6, 7, 2, 3]
                for s4 in range(4):
                    for hh in range(2):
                        bk = ybanks[(s4 * 2 + hh) % 4]

                        def mm2(e, s4=s4, hh=hh, bk=bk):
                            for fc in range(2):
                                ins = e.matmul(banks[bk][:], lhsT=a3[:, fc, s4 * 128:(s4 + 1) * 128],
                                               rhs=W2[:, fc, hh * 512:(hh + 1) * 512], start=(fc == 0), stop=(fc == 1))
                            return ins
                        P.op("pe", mm2, reads=["aT", "w2b%d" % wb], writes=["pb%d" % bk])
                        yv = ys3[:, s4, hh * 512:(hh + 1) * 512]
                        if (s4 * 2 + hh) % 2 == 0:
                            P.op("act", lambda e, bk=bk, yv=yv: e.activation(out=yv, in_=banks[bk][:], func=AF.Identity),
                                 reads=["pb%d" % bk], writes=[ysn])
                        else:
                            P.op("dve", lambda e, bk=bk, yv=yv: e.tensor_copy(out=yv, in_=banks[bk][:]), reads=["pb%d" % bk], writes=[ysn])
                P.dma(lambda e, ys3=ys3, b=b: e.dma_start(out=YS[b * 512:(b + 1) * 512, :].rearrange("(s p) c -> p s c", p=128), in_=ys3),
                      reads=[ysn], writes=["YS_%d" % b])

            weights(0)
            xs_load(0)
            weights(1)
            xs_load(1)
            casts(0)
            prep_half(0, 0)
            prep_half(0, 1)
            for b in range(NBLK):
                nb_ = b + 1
                if b + 2 < NBLK:
                    xs_load(b + 2)
                up_gate(b, 0)
                if nb_ < NBLK:
                    prep_half(nb_, 0)
                up_gate(b, 1)
                if nb_ < NBLK:
                    prep_half(nb_, 1)
                down(b)
                if b + 2 < NBLK:
                    weights(b + 2)
            P.barrier()
            AR.off = mark
            accb = [AR.f32(1024), AR.f32(1024)]
            rows = [AR.f32(1024) for _ in range(8)]
            junk = AR.bf16(1024)
            obuf = [AR.f32(1024), AR.f32(1024)]
            ri = 0
            accb.append(AR.f32(1024))
            P.dma(lambda e: e.dma_start(out=accb[0], in_=X1[0:128, :]), writes=["acc0"])
            for t in range(NT):
                acc = accb[t % 3]; an = "acc%d" % (t % 3)
                if t + 1 < NT:
                    P.dma(lambda e, t=t: e.dma_start(out=accb[(t + 1) % 3], in_=X1[(t + 1) * 128:(t + 2) * 128, :]),
                          writes=["acc%d" % ((t + 1) % 3)])
                for k in range(8):
                    rw = rows[ri % 8]; rn = "row%d" % (ri % 8)
                    ri += 1
                    P.dma(lambda e, rw=rw, t=t, k=k: e.indirect_dma_start(
                        out=rw, out_offset=None, in_=YS[:, :],
                        in_offset=bass.IndirectOffsetOnAxis(ap=DKi3[:, t, k:k + 1], axis=0)),
                        reads=["YS", "DKi"], writes=[rn], eng="pool")
                    P.op("dve", lambda e, rw=rw, acc=acc, t=t, k=k: e.scalar_tensor_tensor(
                        out=acc, in0=rw, scalar=WK3[:, t, k:k + 1], in1=acc, op0=ALU.mult, op1=ALU.add),
                        reads=[rn, an, "WK"], writes=[an])
                ob = obuf[t % 2]; obn = "obuf%d" % (t % 2)
                ssq = stat[:, 48 + (t % 2):49 + (t % 2)]
                sn = "fss%d" % (t % 2)
                P.op("act", lambda e, acc=acc, ssq=ssq: e.activation(out=junk, in_=acc, func=AF.Square, accum_out=ssq),
                     reads=[an], writes=["junk", sn])
                P.op("dve", lambda e, ssq=ssq: e.tensor_scalar(out=ssq, in0=ssq, scalar1=1.0 / D, scalar2=EPS, op0=ALU.mult, op1=ALU.add),
                     reads=[sn], writes=[sn])
                P.op("act", lambda e, ssq=ssq: e.activation(out=ssq, in_=ssq, func=AF.Sqrt), reads=[sn], writes=[sn])
                P.op("dve", lambda e, ssq=ssq: e.reciprocal(out=ssq, in_=ssq), reads=[sn], writes=[sn])
                P.op("dve", lambda e, acc=acc, ob=ob, ssq=ssq: e.scalar_tensor_tensor(
                    out=ob, in0=acc, scalar=ssq, in1=fgbc[:], op0=ALU.mult, op1=ALU.mult),
                    reads=[an, sn, "fgbc"], writes=[obn])
                P.dma(lambda e, ob=ob, t=t: e.dma_start(out=y_d[t * 128:(t + 1) * 128, :], in_=ob), reads=[obn])
            P.barrier()

        LN8c = sb("LN8c", [128, 1])
        EPSc = sb("EPSc", [128, 1])
        P.op("dve", lambda e: e.memset(LN8c[:], LN8), writes=["LN8c"])
        P.op("dve", lambda e: e.memset(EPSc[:], EPS), writes=["EPSc"])
        phase0()
        if upto >= 1:
            phase1()
        if upto >= 2:
            phase2_attention()
        if upto >= 3:
            phase3_retention()
        if upto >= 4:
            phase4()
        if upto >= 5:
            phase5()
        P.barrier()
        P.emit()
    return nc


def _rope_tables():
    n_freq = 16
    inv = (10000.0 ** (-np.arange(n_freq, dtype=np.float32) / n_freq)).astype(np.float32)
    t = np.arange(SEQ)
    rows = (t // 64).astype(np.float32)
    cols = (t % 64).astype(np.float32)
    ang = np.stack([rows[:, None] * inv[None], cols[:, None] * inv[None]], axis=1).astype(np.float32)
    c = np.cos(ang).astype(np.float32)
    s = np.sin(ang).astype(np.float32)
    C = np.zeros((64, SEQ), np.float32)
    S = np.zeros((64, SEQ), np.float32)
    for ax in range(2):
        for hf in range(2):
            C[ax * 32 + hf * 16:ax * 32 + hf * 16 + 16] = c[:, ax, :].T
            S[ax * 32 + hf * 16:ax * 32 + hf * 16 + 16] = s[:, ax, :].T
    return np.concatenate([C, C], 0), np.concatenate([S, S], 0)


def _const_tables():
    j = np.arange(128, dtype=np.float32)[:, None]
    i = np.arange(512, dtype=np.float32)[None, :]
    idxe = (i - j).astype(np.float32)
    df = []
    db = []
    for s in range(4):
        a = i - 128 * s - j
        df.append(np.where(a >= 0, a, BIG))
        b = 128 * s + j - i
        db.append(np.where(b >= 0, b, BIG))
    diagf = np.concatenate(df, 1).astype(np.float32)
    diagb = np.concatenate(db, 1).astype(np.float32)
    iota = np.tile((128.0 * np.arange(32, dtype=np.float32))[None], (128, 1))
    return idxe, diagf, diagb, iota


def _offsets(hf):
    offo = np.zeros((NQB, 32), np.float32)
    offcf = np.zeros((NQB, 2), np.float32)
    offcb = np.zeros((NQB, 2), np.float32)
    for qb in range(NQB):
        n0 = hf * HALF + 512 * qb
        for cc in range(2):
            c0 = 128 * cc
            offcf[qb, cc] = n0 + 256 - c0
            offcb[qb, cc] = SEQ - n0 + c0
        for ko in range(32):
            m0 = (1 - hf) * HALF + 128 * ko
            offo[qb, ko] = abs(n0 - m0)
    t = lambda a: np.tile(a.reshape(1, -1), (128, 1)).astype(np.float32)
    selv = np.tile(np.array([[1.0, 0.0]] if hf == 1 else [[0.0, 1.0]], np.float32), (128, 1))
    return t(offo), t(offcf), t(offcb), selv


def make_in_maps(inp):
    f = lambda a: np.ascontiguousarray(np.asarray(a, dtype=np.float32))
    x = f(inp["x"]); c = f(inp["c"]); ctx = f(inp["ctx"]); c_ctx = f(inp["c_ctx"])
    C, S = _rope_tables()
    idxe, diagf, diagb, iota = _const_tables()
    ident = np.eye(128, dtype=np.float32).astype(ml_dtypes.bfloat16)
    fm = lambda v: np.ascontiguousarray(v.reshape(8, 128).T)
    def lay13(w):
        E = w.shape[0]
        return np.ascontiguousarray(w.reshape(E, 8, 128, 256).transpose(0, 2, 1, 3).reshape(E * 128, 2048))

    def lay2(w):
        E = w.shape[0]
        return np.ascontiguousarray(w.reshape(E, 2, 128, 1024).transpose(0, 2, 1, 3).reshape(E * 128, 2048))
    e_w1 = lay13(np.concatenate([f(inp["exp_w1"])[0], f(inp["shared_w1"])], 0))
    e_w3 = lay13(np.concatenate([f(inp["exp_w3"])[0], f(inp["shared_w3"])], 0))
    e_w2 = lay2(np.concatenate([f(inp["exp_w2"])[0], f(inp["shared_w2"])], 0))
    kk = np.arange(128)
    utri = (kk[:, None] < kk[None, :]).astype(np.float32).astype(ml_dtypes.bfloat16)
    onesb = np.ones((128, 128), np.float32).astype(ml_dtypes.bfloat16)
    j512 = np.tile((512.0 * np.arange(8, dtype=np.float32))[None], (128, 1))
    b512 = np.tile((512.0 * np.arange(128, dtype=np.float32))[None], (128, 1))
    iotap = np.arange(128, dtype=np.float32).reshape(128, 1)
    xbase = (128.0 * np.arange(512, dtype=np.float32)[None, :] + np.arange(128, dtype=np.float32)[:, None]).astype(np.float32)
    lamv = np.concatenate([f(inp["lambda_q1"])[0], f(inp["lambda_q2"])[0], f(inp["lambda_k1"])[0], f(inp["lambda_k2"])[0]])
    rdec = np.concatenate([f(inp["ret_decay_fwd"])[0], f(inp["ret_decay_bwd"])[0]])
    shared = {
        "ada_w": f(inp["ada_w"])[0], "ada_b": f(inp["ada_b"]), "n1g": fm(f(inp["norm1_g"])[0]), "n2g": fm(f(inp["norm2_g"])[0]),
        "w_in": f(inp["w_in"])[0], "w_out": f(inp["w_out"])[0], "lamv": lamv, "dng": f(inp["dattn_norm_g"])[0],
        "rng": f(inp["ret_norm_g"])[0], "rdec": rdec, "router_w": f(inp["router_w"])[0], "router_b": f(inp["router_bias"])[0],
        "e_w1": e_w1, "e_w3": e_w3, "e_w2": e_w2, "fng": f(inp["final_norm_g"]), "ident": ident,
        "idxe": idxe, "diagf": diagf, "diagb": diagb, "iota128": iota,
        "utri": utri, "onesb": onesb, "j512": j512, "b512": b512, "iotap": iotap, "xbase": xbase,
        "ioti": np.tile(np.arange(512, dtype=np.float32)[None], (128, 1)),
    }
    maps = []
    one = np.ones((128, CTX), np.float32)
    zero = np.zeros((128, CTX), np.float32)
    for core in range(8):
        b, hf = core // 2, core % 2
        own = slice(hf * HALF, (hf + 1) * HALF)
        oth = slice((1 - hf) * HALF, (2 - hf) * HALF)
        m = dict(shared)
        m["xtok"] = np.ascontiguousarray(np.concatenate([ctx[b], x[b, own], x[b, oth]], 0))
        c2 = np.stack([fm(c[b]), fm(c_ctx)], axis=2).reshape(128, 16)
        m["c2"] = np.ascontiguousarray(c2)
        m["ropeC"] = np.ascontiguousarray(np.concatenate([one, C[:, own], C[:, oth]], 1))
        m["ropeS"] = np.ascontiguousarray(np.concatenate([zero, S[:, own], S[:, oth]], 1))
        m["offo"], m["offcf"], m["offcb"], m["selv"] = _offsets(hf)
        maps.append(m)
    return maps


def kernel(**inputs):
    nc = build_program()
    in_maps = make_in_maps(inputs)
    res = run_bass_kernel_spmd(nc, in_maps, core_ids=list(range(8)))
    out = np.zeros((4, SEQ, D), np.float32)
    for core in range(8):
        b, hf = core // 2, core % 2
        out[b, hf * HALF:(hf + 1) * HALF] = np.asarray(res.results[core]["y"], dtype=np.float32)
    return out
```

```python
import contextlib
import math
import numpy as np
import ml_dtypes
import concourse.bass as bass
import concourse.mybir as mybir
from concourse.bass_utils import run_bass_kernel_spmd

F32 = mybir.dt.float32
BF16 = mybir.dt.bfloat16
ALU = mybir.AluOpType
AF = mybir.ActivationFunctionType
AX = mybir.AxisListType

D = 1024
SEQ = 8192
HALF = 4096
CTX = 256
NTOK = CTX + SEQ
NKC = NTOK // 128
NQB = HALF // 512
NE = 64
BIG = 1.0e6
LN8 = math.log(0.125)
EPS = 1e-6


class Prog:
    CE = ("act", "pool", "pe", "dve")
    NDMA = 32

    def __init__(self, nc):
        self.nc = nc
        self.ops = {e: [] for e in ("sync",) + self.CE}
        self.cnt = {e: 0 for e in self.CE}
        self.seen = {e: {} for e in ("sync",) + self.CE}
        self.res = {}
        self.dma_cnt = [0] * self.NDMA
        self.dma_rr = 0
        self.dma_rr_q = {}

    def _deps(self, eng, reads, writes):
        toks = {}

        def add(t):
            if t is None:
                return
            k, v = t
            if eng == "pe" and k == "pe":
                return
            if toks.get(k, 0) < v:
                toks[k] = v
        for r in reads:
            st = self.res.get(r)
            if st is not None:
                add(st["w"])
        for w in writes:
            st = self.res.get(w)
            if st is not None:
                add(st["w"])
                for k, v in st["r"].items():
                    add((k, v))
        out = []
        for k, v in toks.items():
            if self.seen[eng].get(k, 0) < v:
                self.seen[eng][k] = v
                out.append((k, v))
        return out

    def _commit(self, tok, reads, writes):
        k, v = tok
        for r in reads:
            st = self.res.setdefault(r, {"w": None, "r": {}})
            if st["r"].get(k, 0) < v:
                st["r"][k] = v
        for w in writes:
            self.res[w] = {"w": tok, "r": {}}

    def op(self, eng, fn, reads=(), writes=()):
        waits = self._deps(eng, reads, writes)
        self.cnt[eng] += 1
        tok = (eng, self.cnt[eng])
        self.ops[eng].append((waits, fn, eng, 1))
        self._commit(tok, reads, writes)

    def dma(self, fn, reads=(), writes=(), eng="sync"):
        half = self.NDMA // 2
        rr = self.dma_rr_q.setdefault(eng, 0)
        self.dma_rr_q[eng] = (rr + 1) % half
        s = rr + (0 if eng == "sync" else half)
        key = ("dma", s)
        waits = self._deps(eng, reads, writes)
        prev = self.dma_cnt[s]
        if prev > 0 and self.seen[eng].get(key, 0) < prev:
            self.seen[eng][key] = prev
            waits.append((key, prev))
        self.dma_cnt[s] = prev + 16
        tok = (key, prev + 16)
        self.ops[eng].append((waits, fn, key, 16))
        self._commit(tok, reads, writes)

    def barrier(self):
        allw = []
        for s in range(self.NDMA):
            if self.dma_cnt[s] > 0:
                allw.append((("dma", s), self.dma_cnt[s]))
        for e in self.CE:
            if self.cnt[e] > 0:
                allw.append((e, self.cnt[e]))
        for eng in ("sync",) + self.CE:
            waits = []
            for k, v in allw:
                if self.seen[eng].get(k, 0) < v:
                    self.seen[eng][k] = v
                    waits.append((k, v))
            if waits:
                self.ops[eng].append((waits, None, None, 0))
        self.res = {}

    def emit(self):
        nc = self.nc
        with contextlib.ExitStack() as st:
            sems = {}
            for e in self.CE:
                sems[e] = st.enter_context(nc.semaphore("s_" + e))
            for s in range(self.NDMA):
                sems[("dma", s)] = st.enter_context(nc.semaphore("s_dma%d" % s))
            block = st.enter_context(nc.Block())

            def run(engname):
                def body(eng):
                    for waits, fn, inc, amt in self.ops[engname]:
                        for k, v in waits:
                            eng.wait_ge(sems[k], v)
                        if fn is not None:
                            ins = fn(eng)
                            ins.then_inc(sems[inc], amt)
                return body
            block.sync(run("sync"))
            block.scalar(run("act"))
            block.gpsimd(run("pool"))
            block.tensor(run("pe"))
            block.vector(run("dve"))


class Arena:
    def __init__(self, ap, nwords):
        self.ap = ap
        self.n = nwords
        self.off = 0

    def reset(self):
        self.off = 0

    def f32(self, n):
        a = self.ap[:, self.off:self.off + n]
        self.off += n
        assert self.off <= self.n, ("arena overflow", self.off)
        return a

    def bf16(self, n):
        assert n % 2 == 0
        return self.f32(n // 2).bitcast(BF16)


def build_program(debug=False, upto=9):
    nc = bass.Bass("TRN2", target_bir_lowering=False)
    okind = "ExternalOutput" if debug else "Internal"

    def din(name, shape, dt=F32):
        return nc.dram_tensor(name, list(shape), dt, kind="ExternalInput").ap()

    xtok = din("xtok", [NTOK, D])
    c2_d = din("c2", [128, 16])
    adaw_d = din("ada_w", [D, 6 * D])
    adab_d = din("ada_b", [1, 6 * D])
    n1g_d = din("n1g", [128, 8])
    n2g_d = din("n2g", [128, 8])
    win_d = din("w_in", [D, 3072])
    wout_d = din("w_out", [D, D])
    lam_d = din("lamv", [256])
    dng_d = din("dng", [128])
    rng_d = din("rng", [128])
    rdec_d = din("rdec", [8])
    rw_d = din("router_w", [D, NE])
    rb_d = din("router_b", [NE])
    ew1_d = din("e_w1", [(NE + 1) * 128, 2048])
    ew3_d = din("e_w3", [(NE + 1) * 128, 2048])
    ew2_d = din("e_w2", [(NE + 1) * 128, 2048])
    utri_d = din("utri", [128, 128], BF16)
    onesb_d = din("onesb", [128, 128], BF16)
    j512_d = din("j512", [128, 8])
    b512_d = din("b512", [128, 128])
    iotap_d = din("iotap", [128, 1])
    xbase_d = din("xbase", [128, 512])
    ioti_d = din("ioti", [128, 512])
    fng_d = din("fng", [D])
    ident_d = din("ident", [128, 128], BF16)
    ropeC_d = din("ropeC", [128, NTOK])
    ropeS_d = din("ropeS", [128, NTOK])
    idxe_d = din("idxe", [128, 512])
    diagf_d = din("diagf", [128, 2048])
    diagb_d = din("diagb", [128, 2048])
    iota_d = din("iota128", [128, 32])
    offo_d = din("offo", [128, NQB * 32])
    offcf_d = din("offcf", [128, NQB * 2])
    offcb_d = din("offcb", [128, NQB * 2])
    selv_d = din("selv", [128, 2])
    y_d = nc.dram_tensor("y", [HALF, D], F32, kind="ExternalOutput").ap()

    QT = nc.dram_tensor("QT", [768, HALF], BF16, kind=okind).ap()
    KT = nc.dram_tensor("KT", [768, NTOK], BF16, kind=okind).ap()
    VS = nc.dram_tensor("VS", [NTOK, 1024], BF16, kind=okind).ap()
    RGT = nc.dram_tensor("RGT", [512, HALF], F32, kind=okind).ap()
    CATT = nc.dram_tensor("CATT", [512, HALF], BF16, kind=okind).ap()
    CAT = nc.dram_tensor("CAT", [HALF, 1024], BF16, kind=okind).ap()
    X1 = nc.dram_tensor("X1", [HALF, D], F32, kind=okind).ap()
    XN = nc.dram_tensor("XN", [HALF, D], BF16, kind=okind).ap()
    EWB = [nc.dram_tensor("EWB%d" % i, [NE * 128, 2048], BF16, kind="Internal").ap() for i in range(3)]
    NSLOT = (HALF * 8 // 512 + NE) * 512
    XS = nc.dram_tensor("XS", [NSLOT, D], BF16, kind="Internal").ap()
    YS = nc.dram_tensor("YS", [NSLOT, D], F32, kind="Internal").ap()
    if debug:
        dbg_dk = nc.dram_tensor("dbg_dk", [128, 256], F32, kind="ExternalOutput").ap()
        dbg_wk = nc.dram_tensor("dbg_wk", [128, 256], F32, kind="ExternalOutput").ap()
        dbg_eb = nc.dram_tensor("dbg_eb", [128, 128], F32, kind="ExternalOutput").ap()

    with contextlib.ExitStack() as st:
        def sb(name, shape, dt=F32):
            return st.enter_context(nc.sbuf_tensor(name, list(shape), dt))

        NAR = 37000
        arena_t = sb("arena", [128, NAR])
        AR = Arena(arena_t, NAR)
        ident = sb("ident_sb", [128, 128], BF16)
        Ax = sb("Ax", [128, 8]); Bx = sb("Bx", [128, 8])
        Ac = sb("Ac", [128, 8]); Bc = sb("Bc", [128, 8])
        A2 = sb("A2", [128, 8]); B2 = sb("B2", [128, 8])
        g1bc = sb("g1bc", [128, D]); g2bc = sb("g2bc", [128, D]); fgbc = sb("fgbc", [128, D])
        neglam = sb("neglam", [128, 1])
        again = sb("again", [128, 128]); rgain = sb("rgain", [128, 128])
        lg = sb("lg", [128, 8]); nlg = sb("nlg", [128, 8])
        rbias = sb("rbias", [128, NE])
        stat = sb("stat", [128, 64])
        SELM = sb("SELM", [128, 32 * NE]); GWt = sb("GWt", [128, 32 * NE]); TOP8 = sb("TOP8", [128, 32 * 8])
        Mall = sb("Mall", [128, 32 * NE], BF16)
        cvf = [sb("cvf0", [128, 2048]), sb("cvf1", [128, 2048])]
        cvb = [sb("cvb0", [128, 2048], BF16), sb("cvb1", [128, 2048], BF16)]
        cvi = {"i": 0}

        def emit_preconv(e_lo, e_hi):
            for ex in range(e_lo, e_hi):
                for mi, tab in enumerate((ew1_d, ew3_d, ew2_d)):
                    i = cvi["i"] % 2
                    cvi["i"] += 1
                    P.dma(lambda e, tab=tab, ex=ex, i=i: e.dma_start(out=cvf[i][:], in_=tab[ex * 128:(ex + 1) * 128, :]),
                          writes=["cvf%d" % i], eng="pool")
                    if mi < 2:
                        P.op("pool", lambda e, i=i: e.tensor_copy(out=cvb[i][:], in_=cvf[i][:]), reads=["cvf%d" % i], writes=["cvb%d" % i])
                    else:
                        for k2 in range(2):
                            P.op("pool", lambda e, i=i, k2=k2: e.tensor_tensor(
                                out=cvb[i][:, k2 * 1024:(k2 + 1) * 1024], in0=cvf[i][:, k2 * 1024:(k2 + 1) * 1024], in1=g2bc[:], op=ALU.mult),
                                reads=["cvf%d" % i, "g2bc"], writes=["cvb%d" % i])
                    P.dma(lambda e, mi=mi, ex=ex, i=i: e.dma_start(out=EWB[mi][ex * 128:(ex + 1) * 128, :], in_=cvb[i][:]),
                          reads=["cvb%d" % i], writes=["EWB%d_%d" % (mi, ex)], eng="pool")
        psum_all = st.enter_context(nc.psum_tensor("psum_all", [128, 4096], F32))
        banks = [psum_all[:, i * 512:(i + 1) * 512] for i in range(8)]
        P = Prog(nc)

        def phase0():
            AR.reset()
            c2 = AR.f32(16)
            c2s = AR.f32(16)
            c2bc = AR.f32(8 * 128)
            ones_row = AR.f32(128)
            adab = AR.f32(6 * D)
            modT = AR.f32(96)
            adaw = [AR.f32(8 * 1024), AR.f32(8 * 1024)]
            n1g = AR.f32(8); n2g = AR.f32(8)
            lamt = AR.f32(256); lamp = AR.f32(128); lams = AR.f32(2)
            rdec = AR.f32(8)
            c2s3 = c2s.rearrange("p (k t) -> p k t", t=2)
            c2bc3 = c2bc.rearrange("p (k n) -> p k n", n=128)
            modT3 = modT.rearrange("p (j t) -> p j t", t=2)
            modps = banks[0][:, 0:96].rearrange("p (j t) -> p j t", t=2)

            P.dma(lambda e: e.dma_start(out=ident[:], in_=ident_d), writes=["ident"])
            P.dma(lambda e: e.dma_start(out=c2, in_=c2_d), writes=["c2"])
            P.dma(lambda e: e.dma_start(out=adab[0:1, :], in_=adab_d), writes=["adab"])
            P.dma(lambda e: e.dma_start(out=n1g, in_=n1g_d), writes=["n1g"])
            P.dma(lambda e: e.dma_start(out=n2g, in_=n2g_d), writes=["n2g"])
            P.dma(lambda e: e.dma_start(out=fgbc[:], in_=fng_d.partition_broadcast(128)), writes=["fgbc"])
            P.dma(lambda e: e.dma_start(out=lamt, in_=lam_d.partition_broadcast(128)), writes=["lamt"])
            P.dma(lambda e: e.dma_start(out=again[:], in_=dng_d.partition_broadcast(128)), writes=["again"])
            P.dma(lambda e: e.dma_start(out=rgain[:], in_=rng_d.partition_broadcast(128)), writes=["rgain"])
            P.dma(lambda e: e.dma_start(out=rdec, in_=rdec_d.partition_broadcast(128)), writes=["rdec"])
            P.dma(lambda e: e.dma_start(out=rbias[:], in_=rb_d.partition_broadcast(128)), writes=["rbias"])
            P.op("dve", lambda e: e.memset(ones_row, 1.0), writes=["ones"])
            P.op("act", lambda e: e.activation(out=c2s, in_=c2, func=AF.Silu), reads=["c2"], writes=["c2s"])
            P.op("dve", lambda e: e.tensor_copy(out=c2bc3, in_=c2s3[:, :, 0:1].to_broadcast([128, 8, 128])),
                 reads=["c2s"], writes=["c2bc"])
            P.op("dve", lambda e: e.tensor_scalar(out=again[:], in0=again[:], scalar1=0.8, scalar2=None, op0=ALU.mult),
                 reads=["again"], writes=["again"])
            lt = lamt.rearrange("p (a n) -> p a n", n=64)
            lp = lamp.rearrange("p (a n) -> p a n", n=64)
            P.op("dve", lambda e: e.tensor_tensor(out=lp, in0=lt[:, 0:2, :], in1=lt[:, 2:4, :], op=ALU.mult),
                 reads=["lamt"], writes=["lamp"])
            P.op("dve", lambda e: e.tensor_reduce(out=lams, in_=lp, axis=AX.X, op=ALU.add), reads=["lamp"], writes=["lams"])
            P.op("act", lambda e: e.activation(out=lams, in_=lams, func=AF.Exp), reads=["lams"], writes=["lams"])
            P.op("dve", lambda e: e.scalar_tensor_tensor(out=neglam[:], in0=lams[:, 1:2], scalar=-0.2, in1=lams[:, 0:1],
                                                          op0=ALU.add, op1=ALU.subtract),
                 reads=["lams"], writes=["neglam"])
            P.op("act", lambda e: e.activation(out=lg[:], in_=rdec, func=AF.Exp, scale=-1.0), reads=["rdec"], writes=["lg"])
            P.op("dve", lambda e: e.tensor_scalar(out=lg[:], in0=lg[:], scalar1=1.0, scalar2=None, op0=ALU.add),
                 reads=["lg"], writes=["lg"])
            P.op("act", lambda e: e.activation(out=nlg[:], in_=lg[:], func=AF.Ln), reads=["lg"], writes=["nlg"])
            P.op("dve", lambda e: e.tensor_scalar(out=lg[:], in0=nlg[:], scalar1=-1.0, scalar2=None, op0=ALU.mult),
                 reads=["nlg"], writes=["lg"])

            for q in range(6):
                aw = adaw[q % 2]
                aw3 = aw.rearrange("p (k c) -> p k c", c=1024)
                P.dma(lambda e, q=q, aw3=aw3: e.dma_start(
                    out=aw3, in_=adaw_d[:, q * 1024:(q + 1) * 1024].rearrange("(k p) c -> p k c", p=128)),
                    writes=["adaw%d" % (q % 2)])

                def mm(e, q=q, aw3=aw3):
                    for jj in range(8):
                        j = q * 8 + jj
                        for kc in range(8):
                            e.matmul(modps[:, j, :], lhsT=aw3[:, kc, jj * 128:(jj + 1) * 128], rhs=c2s3[:, kc, :],
                                     start=(kc == 0), stop=False)
                        ins = e.matmul(modps[:, j, :], lhsT=adab[0:1, j * 128:(j + 1) * 128], rhs=ones_row[0:1, 0:2],
                                       start=False, stop=True)
                    return ins
                P.op("pe", mm, reads=["adaw%d" % (q % 2), "c2s", "adab", "ones"], writes=["modps"])
                if q in (2, 5):
                    gdst = g1bc if q == 2 else g2bc

                    def mmb(e, q=q, aw3=aw3):
                        for hh in range(2):
                            for kc in range(8):
                                e.matmul(banks[1 + hh][:], lhsT=c2bc3[:, kc, :], rhs=aw3[:, kc, hh * 512:(hh + 1) * 512],
                                         start=(kc == 0), stop=False)
                            ins = e.matmul(banks[1 + hh][:], lhsT=ones_row[0:1, 0:128],
                                           rhs=adab[0:1, q * 1024 + hh * 512:q * 1024 + (hh + 1) * 512],
                                           start=False, stop=True)
                        return ins
                    P.op("pe", mmb, reads=["adaw%d" % (q % 2), "c2bc", "adab", "ones", "gdst%d" % q], writes=["gps"])
                    for hh in range(2):
                        P.op("dve", lambda e, hh=hh, gdst=gdst: e.tensor_copy(out=gdst[:, hh * 512:(hh + 1) * 512], in_=banks[1 + hh][:]),
                             reads=["gps"], writes=["gdst%d" % q])
            P.op("dve", lambda e: e.tensor_copy(out=modT, in_=banks[0][:, 0:96]), reads=["modps"], writes=["modT"])
            for (Adst, Bdst, jsc, jsh, col, gsrc, nm) in ((Ax, Bx, 8, 0, 0, n1g, "x"), (Ac, Bc, 8, 0, 1, n1g, "c"),
                                                          (A2, B2, 32, 24, 0, n2g, "2")):
                P.op("dve", lambda e, Adst=Adst, jsc=jsc, col=col, gsrc=gsrc: e.scalar_tensor_tensor(
                    out=Adst[:], in0=modT3[:, jsc:jsc + 8, col], scalar=1.0, in1=gsrc, op0=ALU.add, op1=ALU.mult),
                    reads=["modT", "n1g", "n2g"], writes=["A" + nm])
                P.op("dve", lambda e, Bdst=Bdst, jsh=jsh, col=col: e.tensor_copy(out=Bdst[:], in_=modT3[:, jsh:jsh + 8, col]),
                     reads=["modT"], writes=["B" + nm])
            P.barrier()

        def norm_block(nt, load_fn, xts, xns, junk, hT3, Acol, Bcol, tag, after_xn=None, hname=None):
            N = nt * 128
            PB = ["pb0", "pb1", "pb2", "pb3"]
            for t in range(nt):
                xt = xts[t % len(xts)]
                xn = xns[t % len(xns)]
                xr = "%s_x%d" % (tag, t % len(xts))
                nr = "%s_n%d" % (tag, t % len(xns))
                load_fn(t, xt, xr)
                ssq = stat[:, t:t + 1]
                rs = stat[:, 8 + t:9 + t]
                P.op("act", lambda e, xt=xt, ssq=ssq: e.activation(out=junk, in_=xt, func=AF.Square, accum_out=ssq),
                     reads=[xr], writes=["junk", "ss%d" % t])
                P.op("dve", lambda e, ssq=ssq, rs=rs: e.tensor_scalar(out=rs, in0=ssq, scalar1=1.0 / D, scalar2=EPS,
                                                                      op0=ALU.mult, op1=ALU.add),
                     reads=["ss%d" % t], writes=["rs%d" % t])
                P.op("act", lambda e, rs=rs: e.activation(out=rs, in_=rs, func=AF.Sqrt), reads=["rs%d" % t], writes=["rs%d" % t])
                P.op("dve", lambda e, rs=rs: e.reciprocal(out=rs, in_=rs), reads=["rs%d" % t], writes=["rs%d" % t])
                P.op("dve", lambda e, xt=xt, xn=xn, rs=rs: e.tensor_scalar(out=xn, in0=xt, scalar1=rs, scalar2=None, op0=ALU.mult),
                     reads=[xr, "rs%d" % t], writes=[nr])
                if after_xn is not None:
                    after_xn(t, xn, nr)

                def tr(e, xn=xn, t=t):
                    for kc in range(8):
                        pv = banks[kc // 2][:].bitcast(BF16)
                        ins = e.transpose(out=pv[:, (kc % 2) * 512 + t * 128:(kc % 2) * 512 + (t + 1) * 128],
                                          in_=xn[:, kc * 128:(kc + 1) * 128], identity=ident[:])
                    return ins
                P.op("pe", tr, reads=[nr, "ident"], writes=PB)
            for kc in range(8):
                pv = banks[kc // 2][:].bitcast(BF16)
                P.op("act", lambda e, kc=kc, pv=pv: e.activation(
                    out=hT3[:, kc, 0:N], in_=pv[:, (kc % 2) * 512:(kc % 2) * 512 + N], func=AF.Identity,
                    scale=Acol[:, kc:kc + 1], bias=Bcol[:, kc:kc + 1]),
                    reads=[PB[kc // 2]], writes=[hname or ("hT_" + tag)])

        def phase1():
            AR.reset()
            Wb = AR.bf16(8 * 3072)
            Wr = AR.bf16(8 * 1536)
            wst = [AR.f32(8 * 256), AR.f32(8 * 256)]
            xts = [AR.f32(1024), AR.f32(1024)]
            xns = [AR.bf16(1024), AR.bf16(1024)]
            junk = AR.bf16(1024)
            hTs = [AR.bf16(8 * 512), AR.bf16(8 * 512)]
            cosbs = [AR.f32(512), AR.f32(512)]; sinbs = [AR.f32(512), AR.f32(512)]
            t1s = [AR.f32(512), AR.f32(512)]; t2s = [AR.f32(512), AR.f32(512)]
            ofm = [AR.bf16(512), AR.bf16(512)]
            vt = [AR.bf16(1024), AR.bf16(1024)]
            rgt = [AR.f32(512), AR.f32(512)]
            Wb3 = Wb.rearrange("p (k c) -> p k c", c=3072)
            Wr3 = Wr.rearrange("p (k c) -> p k c", c=1536)
            hT3s = [h_.rearrange("p (k n) -> p k n", n=512) for h_ in hTs]
            for q in range(12):
                ws = wst[q % 2].rearrange("p (k c) -> p k c", c=256)
                P.dma(lambda e, q=q, ws=ws: e.dma_start(
                    out=ws, in_=win_d[:, q * 256:(q + 1) * 256].rearrange("(k p) c -> p k c", p=128)),
                    writes=["wst%d" % (q % 2)])
                P.op("dve", lambda e, q=q, ws=ws: e.tensor_copy(out=Wb3[:, :, q * 256:(q + 1) * 256], in_=ws),
                     reads=["wst%d" % (q % 2)], writes=["Wb"])
            for gi, (c0, ncol) in enumerate(((0, 1024), (1536, 512))):
                r0 = 0 if gi == 0 else 1024
                for kc in range(8):
                    src = Wb3[:, kc, c0:c0 + ncol].rearrange("p (g h f) -> p g h f", h=2, f=16)
                    dst = Wr3[:, kc, r0:r0 + ncol].rearrange("p (g h f) -> p g h f", h=2, f=16)
                    P.op("dve", lambda e, src=src, dst=dst: e.tensor_scalar(
                        out=dst[:, :, 0, :], in0=src[:, :, 1, :], scalar1=-1.0, scalar2=None, op0=ALU.mult),
                        reads=["Wb"], writes=["Wr"])
                    P.op("act", lambda e, src=src, dst=dst: e.activation(out=dst[:, :, 1, :], in_=src[:, :, 0, :], func=AF.Identity),
                         reads=["Wb"], writes=["Wr"])
            fm = []
            for i in range(4):
                fm.append((i * 128, i * 128, QT, i * 128, True))
            for i in range(4):
                fm.append((512 + i * 128, 512 + i * 128, KT, i * 128, False))
            for i in range(2):
                fm.append((1536 + i * 128, 1024 + i * 128, QT, 512 + i * 128, True))
            for i in range(2):
                fm.append((1792 + i * 128, 1280 + i * 128, KT, 512 + i * 128, False))
            blocks = [(0, 2, "ctx")] + [(CTX + i * 512, 4, "own") for i in range(8)] + \
                     [(CTX + HALF + i * 512, 4, "oth") for i in range(8)]
            st1 = {"cnt": 0, "tmc": 0, "tmb": 0}

            def do_norm(bi):
                tok0, nt, kind = blocks[bi]
                N = nt * 128
                cosb = cosbs[bi % 2]; sinb = sinbs[bi % 2]

                def load(t, xt, xr, tok0=tok0):
                    P.dma(lambda e: e.dma_start(out=xt, in_=xtok[tok0 + t * 128:tok0 + (t + 1) * 128, :]), writes=[xr])
                norm_block(nt, load, xts, xns, junk, hT3s[bi % 2], Ac if kind == "ctx" else Ax, Bc if kind == "ctx" else Bx, "p1",
                           hname="hT_p1_%d" % (bi % 2))
                P.dma(lambda e: e.dma_start(out=cosb[:, 0:N], in_=ropeC_d[:, tok0:tok0 + N]), writes=["cosb%d" % (bi % 2)])
                P.dma(lambda e: e.dma_start(out=sinb[:, 0:N], in_=ropeS_d[:, tok0:tok0 + N]), writes=["sinb%d" % (bi % 2)])

            def do_proj(bi, part):
                tok0, nt, kind = blocks[bi]
                N = nt * 128
                hT3 = hT3s[bi % 2]
                cosb = cosbs[bi % 2]; sinb = sinbs[bi % 2]
                hn = "hT_p1_%d" % (bi % 2); cn = "cosb%d" % (bi % 2); sn_ = "sinb%d" % (bi % 2)
                cnt = st1["cnt"]; tmc = st1["tmc"]; tmb = st1["tmb"]
                for (wc, rc, dst, drow, own_only) in (fm if part == "fm" else []):
                    if own_only and kind != "own":
                        continue

                    ba, bb = (4, 5) if cnt % 2 == 0 else (6, 7)
                    t1 = t1s[cnt % 2]; t2 = t2s[cnt % 2]
                    t1n = "t1_%d" % (cnt % 2); t2n = "t2_%d" % (cnt % 2)

                    def mm(e, wc=wc, rc=rc, N=N, ba=ba, bb=bb):
                        for kc in range(8):
                            e.matmul(banks[ba][:, 0:N], lhsT=Wb3[:, kc, wc:wc + 128], rhs=hT3[:, kc, 0:N],
                                     start=(kc == 0), stop=(kc == 7))
                        for kc in range(8):
                            ins = e.matmul(banks[bb][:, 0:N], lhsT=Wr3[:, kc, rc:rc + 128], rhs=hT3[:, kc, 0:N],
                                           start=(kc == 0), stop=(kc == 7))
                        return ins
                    P.op("pe", mm, reads=["Wb", "Wr", hn], writes=["ps%d" % ba, "ps%d" % bb])
                    P.op("dve", lambda e, N=N, ba=ba, t1=t1: e.tensor_tensor(out=t1[:, 0:N], in0=banks[ba][:, 0:N], in1=cosb[:, 0:N], op=ALU.mult),
                         reads=["ps%d" % ba, cn], writes=[t1n])
                    P.op("dve", lambda e, N=N, bb=bb, t2=t2: e.tensor_tensor(out=t2[:, 0:N], in0=banks[bb][:, 0:N], in1=sinb[:, 0:N], op=ALU.mult),
                         reads=["ps%d" % bb, sn_], writes=[t2n])
                    o = ofm[cnt % 2]
                    on = "ofm%d" % (cnt % 2)
                    cnt += 1
                    P.op("dve", lambda e, o=o, N=N, t1=t1, t2=t2: e.tensor_tensor(out=o[:, 0:N], in0=t1[:, 0:N], in1=t2[:, 0:N], op=ALU.add),
                         reads=[t1n, t2n], writes=[on])
                    if dst is QT:
                        c0 = tok0 - CTX
                    else:
                        c0 = tok0
                    P.dma(lambda e, o=o, dst=dst, drow=drow, c0=c0, N=N: e.dma_start(out=dst[drow:drow + 128, c0:c0 + N], in_=o[:, 0:N]),
                          reads=[on], eng="pool")
                for t in range(nt if part == "tm" else 0):
                    v = vt[tmc % 2]
                    vn = "vt%d" % (tmc % 2)
                    rgb = rgt[tmc % 2]
                    rn = "rgt%d" % (tmc % 2)
                    tmc += 1
                    groups = [(1024, 4 + (tmb % 4), v[:, 0:512], vn, "copy"), (2048, 4 + ((tmb + 1) % 4), v[:, 512:1024], vn, "copy")]
                    tmb += 2
                    for (wc, bk, dst_sb, dn, how) in groups:
                        def mmt(e, wc=wc, bk=bk, t=t):
                            for kc in range(8):
                                ins = e.matmul(banks[bk][:], lhsT=hT3[:, kc, t * 128:(t + 1) * 128], rhs=Wb3[:, kc, wc:wc + 512],
                                               start=(kc == 0), stop=(kc == 7))
                            return ins
                        P.op("pe", mmt, reads=["Wb", hn, "ev%d" % bk], writes=["ps%d" % bk])
                        if how == "copy" and wc == 1024:
                            P.op("dve", lambda e, bk=bk, dst_sb=dst_sb: e.tensor_copy(out=dst_sb, in_=banks[bk][:]),
                                 reads=["ps%d" % bk], writes=[dn, "ev%d" % bk])
                        elif how == "copy":
                            P.op("act", lambda e, bk=bk, dst_sb=dst_sb: e.activation(out=dst_sb, in_=banks[bk][:], func=AF.Identity),
                                 reads=["ps%d" % bk], writes=[dn, "ev%d" % bk])
                        else:
                            P.op("act", lambda e, bk=bk, dst_sb=dst_sb: e.activation(out=dst_sb, in_=banks[bk][:], func=AF.Silu),
                                 reads=["ps%d" % bk], writes=[dn, "ev%d" % bk])
                    r0 = tok0 + t * 128
                    P.dma(lambda e, v=v, r0=r0: e.dma_start(out=VS[r0:r0 + 128, :], in_=v), reads=[vn], eng="pool")
                if kind == "own" and part == "tm":
                    for i in range(4):
                        bk = 4 + (tmb % 4)
                        tmb += 1
                        rgb = rgt[tmc % 2]; rn = "rgt%d" % (tmc % 2)
                        tmc += 1

                        def mmg(e, i=i, bk=bk):
                            for kc in range(8):
                                ins = e.matmul(banks[bk][:], lhsT=Wb3[:, kc, 2560 + i * 128:2560 + (i + 1) * 128], rhs=hT3[:, kc, :],
                                               start=(kc == 0), stop=(kc == 7))
                            return ins
                        P.op("pe", mmg, reads=["Wb", hn], writes=["ps%d" % bk])
                        P.op("act", lambda e, bk=bk, rgb=rgb: e.activation(out=rgb, in_=banks[bk][:], func=AF.Silu),
                             reads=["ps%d" % bk], writes=[rn])
                        c0 = tok0 - CTX
                        P.dma(lambda e, rgb=rgb, i=i, c0=c0: e.dma_start(out=RGT[i * 128:(i + 1) * 128, c0:c0 + 512], in_=rgb), reads=[rn], eng="pool")
                st1["cnt"] = cnt; st1["tmc"] = tmc; st1["tmb"] = tmb

            do_norm(0)
            for bi in range(len(blocks)):
                do_proj(bi, "fm")
                if bi + 1 < len(blocks):
                    do_norm(bi + 1)
                do_proj(bi, "tm")
            P.barrier()

        def load_head_kv(KTh, Vh3, krow0, nrows, vcol0, tg):
            P.dma(lambda e: e.dma_start(out=KTh[0:nrows, :], in_=KT[krow0:krow0 + nrows, :]), writes=["KTh_%d" % tg])
            for g in range(6):
                P.dma(lambda e, g=g: e.dma_start(
                    out=Vh3[:, g * 11:(g + 1) * 11, 0:128],
                    in_=VS[g * 11 * 128:(g + 1) * 11 * 128, vcol0:vcol0 + 128].rearrange("(c p) e -> p c e", p=128)),
                    writes=["Vh%d_%d" % (g, tg)])

        def finalize_norm(o_sb, on, gain, extra_mul, out_bf, obn, idx, stat_junk):
            ssq = stat[:, 16 + idx:17 + idx]
            sn = "fs%d" % idx
            P.op("act", lambda e: e.activation(out=stat_junk, in_=o_sb, func=AF.Square, accum_out=ssq),
                 reads=[on], writes=["sjunk", sn])
            P.op("act", lambda e: e.activation(out=ssq, in_=ssq, func=AF.Ln, scale=1.0 / 128, bias=EPSc[:]), reads=[sn, "EPSc"], writes=[sn])
            P.op("act", lambda e: e.activation(out=ssq, in_=ssq, func=AF.Exp, scale=-0.5), reads=[sn], writes=[sn])
            if extra_mul is None:
                P.op("dve", lambda e: e.scalar_tensor_tensor(out=out_bf, in0=o_sb, scalar=ssq, in1=gain[:], op0=ALU.mult, op1=ALU.mult),
                     reads=[on, sn], writes=[obn])
            else:
                ex, exn = extra_mul
                P.op("dve", lambda e: e.scalar_tensor_tensor(out=o_sb, in0=o_sb, scalar=ssq, in1=gain[:], op0=ALU.mult, op1=ALU.mult),
                     reads=[on, sn], writes=[on])
                P.op("dve", lambda e: e.tensor_tensor(out=out_bf, in0=o_sb, in1=ex, op=ALU.mult),
                     reads=[on, exn], writes=[obn])

        def phase2_attention():
            AR.reset()
            emit_preconv(0, 52)
            KThs = [AR.bf16(NTOK), AR.bf16(NTOK)]
            Vh3s = [AR.bf16(NKC * 130).rearrange("p (c e) -> p c e", e=130) for _ in range(2)]
            QTb = [AR.bf16(512), AR.bf16(512)]
            NPT = 4
            PT = [AR.bf16(1024) for _ in range(NPT)]
            ocp = AR.f32(3 * 387)
            osb = [AR.f32(128), AR.f32(128)]
            obf = [AR.bf16(128), AR.bf16(128)]
            stat_junk = AR.bf16(128)
            for v_ in Vh3s:
                P.op("dve", lambda e, v_=v_: e.memset(v_[:, :, 128:129], 1.0), writes=["Vones"])
            zt = AR.bf16(8192)
            P.op("dve", lambda e: e.memset(zt, 0.0), writes=["zt"])
            zfill = list(range(NSLOT // 1024))

            def emit_zfill(n):
                for _ in range(n):
                    if zfill:
                        i = zfill.pop(0)
                        P.dma(lambda e, i=i: e.dma_start(out=XS[i * 1024:(i + 1) * 1024, :].rearrange("(p r) c -> p (r c)", r=8), in_=zt),
                              reads=["zt"], writes=["XSz%d" % i])
            oslot = {}
            lst = [(m, qs) for m in range(2) for qs in range(4)]
            for i, key in enumerate(lst):
                oslot[key] = (4 + i // 3, (i % 3) * 129)
            st_ = {"pti": 0, "sci": 0, "fin": 0, "kv": 0}

            def emit_qk(kc, qt, qn):
                pr_ = st_["sci"] % 2
                st_["sci"] += 1
                pt = PT[st_["pti"] % NPT]
                ptn = "PT%d" % (st_["pti"] % NPT)
                st_["pti"] += 1

                KTh = KThs[st_["kv"]]

                def qk(e, pr_=pr_, kc=kc, qt=qt, KTh=KTh):
                    for m in range(2):
                        ins = e.matmul(banks[pr_ * 2 + m][:], lhsT=KTh[m * 64:(m + 1) * 64, kc * 128:(kc + 1) * 128],
                                       rhs=qt[m * 64:(m + 1) * 64, :], start=True, stop=True)
                    return ins
                P.op("pe", qk, reads=["KTh_%d" % st_["kv"], qn], writes=["S%d" % pr_])
                P.op("act", lambda e, pr_=pr_, pt=pt: e.activation(out=pt, in_=psum_all[:, pr_ * 1024:(pr_ + 1) * 1024], func=AF.Exp, scale=0.125),
                     reads=["S%d" % pr_], writes=[ptn])
                return (pt, ptn)

            load_head_kv(KThs[0], Vh3s[0], 0, 128, 0, 0)
            for h in range(4):
                st_["kv"] = h % 2
                Vh3 = Vh3s[h % 2]
                vnames = ["Vh%d_%d" % (g_, h % 2) for g_ in range(6)]
                for qb in range(NQB):
                    if qb == 1 and h + 1 < 4:
                        load_head_kv(KThs[(h + 1) % 2], Vh3s[(h + 1) % 2], (h + 1) * 128, 128, (h + 1) * 128, (h + 1) % 2)
                    it = h * NQB + qb
                    qt = QTb[it % 2]
                    qn = "QTb%d" % (it % 2)
                    if it == 0:
                        P.dma(lambda e, qt=qt: e.dma_start(out=qt, in_=QT[0:128, 0:512]), writes=[qn])
                    if it + 1 < 4 * NQB:
                        h2_, qb2_ = divmod(it + 1, NQB)
                        P.dma(lambda e, h2_=h2_, qb2_=qb2_, it=it: e.dma_start(
                            out=QTb[(it + 1) % 2], in_=QT[h2_ * 128:(h2_ + 1) * 128, qb2_ * 512:(qb2_ + 1) * 512]),
                            writes=["QTb%d" % ((it + 1) % 2)])
                    emit_zfill(3)
                    pend = [emit_qk(0, qt, qn), emit_qk(1, qt, qn)]
                    for kc in range(NKC):
                        pts = pend.pop(0)
                        if kc + 2 < NKC:
                            pend.append(emit_qk(kc + 2, qt, qn))

                        def av(e, pts=pts, kc=kc, Vh3=Vh3):
                            for m in range(2):
                                for qs in range(4):
                                    bk, off = oslot[(m, qs)]
                                    ins = e.matmul(banks[bk][:, off:off + 129], lhsT=pts[0][:, m * 512 + qs * 128:m * 512 + (qs + 1) * 128],
                                                   rhs=Vh3[:, kc, 0:129], start=(kc == 0 and off == 0),
                                                   stop=(kc == NKC - 1 and (off == 258 or (bk == 6 and off == 129))))
                            return ins
                        P.op("pe", av, reads=[pts[1], "Vones"] + vnames, writes=["Oacc"])
                    for b3 in range(3):
                        ncol = 387 if b3 < 2 else 258
                        P.op("dve", lambda e, b3=b3, ncol=ncol: e.tensor_copy(out=ocp[:, b3 * 387:b3 * 387 + ncol], in_=banks[4 + b3][:, 0:ncol]),
                             reads=["Oacc"], writes=["ocp"])
                    for qs in range(4):
                        fi = st_["fin"] % 2
                        st_["fin"] += 1
                        o = osb[fi]; on = "osb%d" % fi
                        ob = obf[fi]; obn = "obf%d" % fi
                        b1, o1 = oslot[(0, qs)]
                        b2, o2 = oslot[(1, qs)]
                        c1 = (b1 - 4) * 387 + o1
                        c2 = (b2 - 4) * 387 + o2
                        r1 = stat[:, 24 + fi:25 + fi]
                        r2 = stat[:, 28 + fi:29 + fi]
                        rn = "r12_%d" % fi
                        P.op("dve", lambda e, c1=c1, r1=r1: e.reciprocal(out=r1, in_=ocp[:, c1 + 128:c1 + 129]),
                             reads=["ocp"], writes=[rn + "a"])
                        P.op("dve", lambda e, c2=c2, r2=r2: e.reciprocal(out=r2, in_=ocp[:, c2 + 128:c2 + 129]),
                             reads=["ocp"], writes=[rn + "b"])
                        P.op("dve", lambda e, r2=r2: e.tensor_tensor(out=r2, in0=r2, in1=neglam[:], op=ALU.mult),
                             reads=[rn + "b", "neglam"], writes=[rn + "b"])
                        P.op("dve", lambda e, o=o, c1=c1, r1=r1: e.tensor_scalar(
                            out=o, in0=ocp[:, c1:c1 + 128], scalar1=r1, scalar2=None, op0=ALU.mult),
                            reads=["ocp", rn + "a"], writes=[on])
                        P.op("dve", lambda e, o=o, c2=c2, r2=r2: e.scalar_tensor_tensor(
                            out=o, in0=ocp[:, c2:c2 + 128], scalar=r2, in1=o, op0=ALU.mult, op1=ALU.add),
                            reads=["ocp", rn + "b", on], writes=[on])
                        finalize_norm(o, on, again, None, ob, obn, fi, stat_junk)
                        row0 = qb * 512 + qs * 128
                        P.dma(lambda e, ob=ob, row0=row0, h=h: e.dma_start(out=CAT[row0:row0 + 128, h * 128:(h + 1) * 128], in_=ob),
                              reads=[obn])
            P.barrier()

        def phase3_retention():
            AR.reset()
            emit_preconv(52, NE)
            KThs = [AR.bf16(NTOK)]
            Vh3s = [AR.bf16(NKC * 130).rearrange("p (c e) -> p c e", e=130)]
            QTb = [AR.bf16(512), AR.bf16(512)]
            NAT = 8
            AT = [AR.bf16(512) for _ in range(NAT)]
            ocp = AR.f32(512); sq = AR.f32(512); rs = AR.f32(512); o2 = AR.f32(512)
            obf = [AR.bf16(512), AR.bf16(512)]
            gat = [AR.f32(512), AR.f32(512)]
            ones_f = AR.f32(128); rgcol = AR.f32(1)
            P.op("dve", lambda e: e.memset(ones_f, 1.0), writes=["ones_f"])
            P.dma(lambda e: e.dma_start(out=rgcol, in_=rng_d.rearrange("(p o) -> p o", o=1)), writes=["rgcol"])
            idxe = AR.f32(512)
            diagf = AR.f32(2048); diagb = AR.f32(2048)
            iot = AR.f32(32)
            offo = AR.f32(NQB * 32); offcf = AR.f32(NQB * 2); offcb = AR.f32(NQB * 2)
            selv = AR.f32(2)
            lgo = AR.f32(4); alo = AR.f32(4); tmp4 = AR.f32(4)
            Ef = AR.f32(512); Eb = AR.f32(512); Eo = AR.f32(512)
            Dd = AR.f32(2048); Dt = AR.f32(2048)
            CF = AR.f32(32); CB = AR.f32(32)
            Co = AR.f32(NQB * 32); Ccf = AR.f32(NQB * 2); Ccb = AR.f32(NQB * 2)
            Mc = [[AR.f32(512), AR.f32(512)], [AR.f32(512), AR.f32(512)]]
            ioti = AR.f32(512); jcol = AR.f32(1)
            XF = AR.f32(512); XB = AR.f32(512); XO = AR.f32(512)
            bF = AR.f32(1); bB = AR.f32(1); bO = AR.f32(1)
            SF = AR.f32(32); SB = AR.f32(32); SO = AR.f32(NQB * 32)
            qcp = [[AR.bf16(512) for _ in range(3)] for _ in range(2)]
            for k_ in KThs:
                P.op("dve", lambda e, k_=k_: e.memset(k_[64:128, :], 0.0), writes=["KTz"])
            for qb_ in range(2):
                P.op("dve", lambda e, qb_=qb_: e.memset(QTb[qb_][64:128, :], 0.0), writes=["QTz%d" % qb_])
                for c_ in range(3):
                    P.op("dve", lambda e, qb_=qb_, c_=c_: e.memset(qcp[qb_][c_][64:128, :], 0.0), writes=["qcz%d_%d" % (qb_, c_)])
            P.dma(lambda e: e.dma_start(out=ioti, in_=ioti_d), writes=["ioti"])
            P.dma(lambda e: e.dma_start(out=jcol, in_=iotap_d), writes=["jcol"])
            for (dst, src, nm) in ((idxe, idxe_d, "idxe"), (diagf, diagf_d, "diagf"), (diagb, diagb_d, "diagb"),
                                   (iot, iota_d, "iot"), (offo, offo_d, "offo"), (offcf, offcf_d, "offcf"),
                                   (offcb, offcb_d, "offcb"), (selv, selv_d, "selv")):
                P.dma(lambda e, dst=dst, src=src: e.dma_start(out=dst, in_=src), writes=[nm])
            P.op("dve", lambda e: e.tensor_scalar(out=lgo, in0=lg[:, 0:4], scalar1=selv[:, 0:1], scalar2=None, op0=ALU.mult),
                 reads=["selv", "lg"], writes=["lgo"])
            P.op("dve", lambda e: e.tensor_scalar(out=tmp4, in0=lg[:, 4:8], scalar1=selv[:, 1:2], scalar2=None, op0=ALU.mult),
                 reads=["selv", "lg"], writes=["tmp4"])
            P.op("dve", lambda e: e.tensor_tensor(out=alo, in0=lgo, in1=tmp4, op=ALU.subtract), reads=["lgo", "tmp4"], writes=["alo"])
            P.op("dve", lambda e: e.tensor_tensor(out=lgo, in0=lgo, in1=tmp4, op=ALU.add), reads=["lgo", "tmp4", "alo"], writes=["lgo"])
            st_ = {"ati": 0, "sci": 0, "fin": 0, "evi": 0, "kv": 0}

            def emit_qk_pair(j, qt, qn, qb):
                bks = [(0, 1), (2, 3), (6, 7)][st_["sci"] % 3]
                st_["sci"] += 1

                qsel = []
                for i2 in range(2):
                    kc = 2 * j + i2
                    ko = kc - 2
                    if kc < 2 or (kc < 34 and 4 * qb <= ko <= 4 * qb + 3):
                        qsel.append(qt)
                    elif kc < 34 and ko < 4 * qb:
                        qsel.append(qcp[qb % 2][0])
                    elif kc < 34:
                        qsel.append(qcp[qb % 2][1])
                    else:
                        qsel.append(qcp[qb % 2][2])

                KTh = KThs[st_["kv"]]

                def qk2(e, j=j, qsel=qsel, bks=bks, KTh=KTh):
                    for i2 in range(2):
                        kc = 2 * j + i2
                        ins = e.matmul(banks[bks[i2]][:], lhsT=KTh[:, kc * 128:(kc + 1) * 128], rhs=qsel[i2], start=True, stop=True)
                    return ins
                P.op("pe", qk2, reads=["KTh_%d" % st_["kv"], "KTz", qn, "qcp%d" % (qb % 2)], writes=["S%d" % bks[0], "S%d" % bks[1]])
                return [emit_mask(2 * j + i2, bks[i2], qb) for i2 in range(2)]

            def emit_mask(kc, bk, qb):
                at = AT[st_["ati"] % NAT]
                atn = "AT%d" % (st_["ati"] % NAT)
                st_["ati"] += 1
                ko = kc - 2
                sn = "S%d" % bk
                if kc < 2:
                    mc = Mc[qb % 2][kc]
                    P.op("dve", lambda e, bk=bk, at=at, mc=mc: e.tensor_tensor(out=at, in0=banks[bk][:], in1=mc, op=ALU.mult),
                         reads=[sn, "Mc%d" % (qb % 2)], writes=[atn])
                elif kc < 34 and 4 * qb <= ko <= 4 * qb + 3:
                    s_ = ko - 4 * qb
                    P.op("dve", lambda e, bk=bk, at=at, s_=s_: e.tensor_tensor(
                        out=at, in0=banks[bk][:], in1=Dd[:, s_ * 512:(s_ + 1) * 512], op=ALU.mult),
                        reads=[sn, "Dd"], writes=[atn])
                else:
                    if kc < 34 and ko < 4 * qb:
                        r = 4 * qb - ko
                        scl, scn = SF[:, r:r + 1], "SF"
                    elif kc < 34:
                        r = ko - 4 * qb
                        scl, scn = SB[:, r:r + 1], "SB"
                    else:
                        ci = qb * 32 + (kc - 34)
                        scl, scn = SO[:, ci:ci + 1], "SO"
                    if st_["evi"] % 5 < 3:
                        P.op("act", lambda e, bk=bk, at=at, scl=scl: e.activation(out=at, in_=banks[bk][:], func=AF.Identity, scale=scl),
                             reads=[sn, scn], writes=[atn])
                    else:
                        P.op("dve", lambda e, bk=bk, at=at, scl=scl: e.tensor_scalar(out=at, in0=banks[bk][:], scalar1=scl, scalar2=None, op0=ALU.mult),
                             reads=[sn, scn], writes=[atn])
                    st_["evi"] += 1
                return (at, atn)

            for h in range(4):
                lf = lg[:, h:h + 1]
                lb = lg[:, 4 + h:5 + h]
                nlb = nlg[:, 4 + h:5 + h]
                lo = lgo[:, h:h + 1]
                ao = alo[:, h:h + 1]
                P.op("act", lambda e, lf=lf: e.activation(out=Ef, in_=idxe, func=AF.Exp, scale=lf, bias=LN8c[:]),
                     reads=["idxe", "lg"], writes=["Ef"])
                P.op("act", lambda e, nlb=nlb: e.activation(out=Eb, in_=idxe, func=AF.Exp, scale=nlb, bias=LN8c[:]),
                     reads=["idxe", "lg"], writes=["Eb"])
                P.op("act", lambda e, lf=lf: e.activation(out=Dd, in_=diagf, func=AF.Exp, scale=lf, bias=LN8c[:]),
                     reads=["diagf", "lg"], writes=["Dd"])
                P.op("act", lambda e, lb=lb: e.activation(out=Dt, in_=diagb, func=AF.Exp, scale=lb, bias=LN8c[:]),
                     reads=["diagb", "lg"], writes=["Dt"])
                P.op("dve", lambda e: e.tensor_tensor(out=Dd, in0=Dd, in1=Dt, op=ALU.add), reads=["Dd", "Dt"], writes=["Dd"])
                nlf = nlg[:, h:h + 1]
                P.op("act", lambda e, lf=lf: e.activation(out=XF, in_=ioti, func=AF.Exp, scale=lf), reads=["ioti", "lg"], writes=["XF"])
                P.op("act", lambda e, nlb=nlb: e.activation(out=XB, in_=ioti, func=AF.Exp, scale=nlb), reads=["ioti", "lg"], writes=["XB"])
                P.op("act", lambda e, ao=ao: e.activation(out=XO, in_=ioti, func=AF.Exp, scale=ao), reads=["ioti", "alo"], writes=["XO"])
                P.op("dve", lambda e, nlf=nlf: e.tensor_scalar(out=bF, in0=jcol, scalar1=nlf, scalar2=LN8, op0=ALU.mult, op1=ALU.add),
                     reads=["jcol", "lg"], writes=["bF"])
                P.op("dve", lambda e, lb=lb: e.tensor_scalar(out=bB, in0=jcol, scalar1=lb, scalar2=LN8, op0=ALU.mult, op1=ALU.add),
                     reads=["jcol", "lg"], writes=["bB"])
                P.op("dve", lambda e, ao=ao: e.tensor_scalar(out=bO, in0=jcol, scalar1=ao, scalar2=-1.0, op0=ALU.mult, op1=ALU.mult),
                     reads=["jcol", "alo"], writes=["bO"])
                P.op("dve", lambda e: e.tensor_scalar(out=bO, in0=bO, scalar1=LN8, scalar2=None, op0=ALU.add), reads=["bO"], writes=["bO"])
                P.op("act", lambda e, lf=lf: e.activation(out=SF, in_=iot, func=AF.Exp, scale=lf, bias=bF[:, 0:1]), reads=["iot", "lg", "bF"], writes=["SF"])
                P.op("act", lambda e, lb=lb: e.activation(out=SB, in_=iot, func=AF.Exp, scale=lb, bias=bB[:, 0:1]), reads=["iot", "lg", "bB"], writes=["SB"])
                P.op("act", lambda e, lo=lo: e.activation(out=SO, in_=offo, func=AF.Exp, scale=lo, bias=bO[:, 0:1]), reads=["offo", "lgo", "bO"], writes=["SO"])
                P.op("act", lambda e, lf=lf: e.activation(out=Ccf, in_=offcf, func=AF.Exp, scale=lf), reads=["offcf", "lg"], writes=["Ccf"])
                P.op("act", lambda e, lb=lb: e.activation(out=Ccb, in_=offcb, func=AF.Exp, scale=lb), reads=["offcb", "lg"], writes=["Ccb"])
                load_head_kv(KThs[0], Vh3s[0], 512 + h * 64, 64, 512 + h * 128, 0)
                st_["kv"] = 0
                Vh3 = Vh3s[0]
                vnames = ["Vh%d_0" % g_ for g_ in range(6)]
                for qb in range(NQB):
                    qt = QTb[qb % 2]
                    qn = "QTb%d" % (qb % 2)
                    P.dma(lambda e, qt=qt, h=h, qb=qb: e.dma_start(
                        out=qt[0:64, :], in_=QT[512 + h * 64:512 + (h + 1) * 64, qb * 512:(qb + 1) * 512]), writes=[qn])
                    for ci_, (xt_, xn_) in enumerate(((XF, "XF"), (XB, "XB"), (XO, "XO"))):
                        P.op("dve", lambda e, qt=qt, ci_=ci_, xt_=xt_, qb=qb: e.tensor_tensor(
                            out=qcp[qb % 2][ci_][0:64, :], in0=qt[0:64, :], in1=xt_[0:64, :], op=ALU.mult),
                            reads=[qn, xn_], writes=["qcp%d" % (qb % 2)])
                    for cc in range(2):
                        mc = Mc[qb % 2][cc]
                        ci = qb * 2 + cc
                        P.op("dve", lambda e, mc=mc, ci=ci: e.tensor_scalar(out=mc, in0=Ef, scalar1=Ccf[:, ci:ci + 1], scalar2=None, op0=ALU.mult),
                             reads=["Ef", "Ccf"], writes=["Mc%d" % (qb % 2)])
                        P.op("dve", lambda e, mc=mc, ci=ci: e.scalar_tensor_tensor(
                            out=mc, in0=Eb, scalar=Ccb[:, ci:ci + 1], in1=mc, op0=ALU.mult, op1=ALU.add),
                            reads=["Eb", "Ccb", "Mc%d" % (qb % 2)], writes=["Mc%d" % (qb % 2)])
                    pend = [emit_qk_pair(0, qt, qn, qb), emit_qk_pair(1, qt, qn, qb)]
                    for j in range(NKC // 2):
                        cur = pend.pop(0)
                        if j + 2 < NKC // 2:
                            pend.append(emit_qk_pair(j + 2, qt, qn, qb))

                        def av2(e, cur=cur, j=j, Vh3=Vh3):
                            for i2 in range(2):
                                kc = 2 * j + i2
                                ins = e.matmul(banks[4][:], lhsT=Vh3[:, kc, 0:128], rhs=cur[i2][0],
                                               start=(kc == 0), stop=(kc == NKC - 1))
                            return ins
                        P.op("pe", av2, reads=[cur[0][1], cur[1][1]] + vnames, writes=["Oacc"])
                    fi = st_["fin"] % 2
                    st_["fin"] += 1
                    g = gat[fi]; gn = "gat%d" % fi
                    ob = obf[fi]; obn = "obf%d" % fi
                    P.dma(lambda e, g=g, h=h, qb=qb: e.dma_start(out=g, in_=RGT[h * 128:(h + 1) * 128, qb * 512:(qb + 1) * 512]), writes=[gn])
                    P.op("act", lambda e: e.activation(out=ocp, in_=banks[4][:], func=AF.Identity), reads=["Oacc"], writes=["ocp"])
                    P.op("act", lambda e: e.activation(out=sq, in_=ocp, func=AF.Square), reads=["ocp"], writes=["sq"])
                    P.op("pe", lambda e: e.matmul(banks[5][:], lhsT=ones_f, rhs=sq, start=True, stop=True), reads=["ones_f", "sq"], writes=["ssps"])
                    P.op("act", lambda e: e.activation(out=rs, in_=banks[5][:], func=AF.Ln, scale=1.0 / 128, bias=EPSc[:]),
                         reads=["ssps", "EPSc"], writes=["rs"])
                    P.op("act", lambda e: e.activation(out=rs, in_=rs, func=AF.Exp, scale=-0.5), reads=["rs"], writes=["rs"])
                    P.op("dve", lambda e: e.tensor_tensor(out=o2, in0=ocp, in1=rs, op=ALU.mult), reads=["ocp", "rs"], writes=["o2"])
                    P.op("dve", lambda e, ob=ob, g=g: e.scalar_tensor_tensor(out=ob, in0=o2, scalar=rgcol[:, 0:1], in1=g, op0=ALU.mult, op1=ALU.mult),
                         reads=["o2", "rgcol", gn], writes=[obn])
                    P.dma(lambda e, ob=ob, h=h, qb=qb: e.dma_start(out=CATT[h * 128:(h + 1) * 128, qb * 512:(qb + 1) * 512], in_=ob), reads=[obn])
            P.barrier()

        def phase4():
            AR.reset()
            Wo = AR.bf16(8 * 1024)
            wst = [AR.f32(8 * 512), AR.f32(8 * 512)]
            ct = [AR.bf16(512), AR.bf16(512)]
            cT = [AR.bf16(512), AR.bf16(512)]
            crT = [AR.bf16(4 * 512), AR.bf16(4 * 512)]
            xts = [AR.f32(1024) for _ in range(4)]
            xns = [AR.bf16(1024), AR.bf16(1024)]
            tmp = AR.f32(1024)
            junk = AR.bf16(1024)
            hT = AR.bf16(8 * 512)
            sw1 = AR.bf16(2048); sw3 = AR.bf16(2048); sw2 = AR.bf16(2048)
            sst = AR.f32(2048)
            rwst = AR.f32(8 * NE); rwb = AR.bf16(8 * NE)
            rt = AR.f32(512)
            sg = [AR.f32(512), AR.f32(512)]
            aT = AR.bf16(1024)
            Wo3 = Wo.rearrange("p (k c) -> p k c", c=1024)
            hT3 = hT.rearrange("p (k n) -> p k n", n=512)
            W1 = sw1.rearrange("p (k f) -> p k f", f=256)
            W3 = sw3.rearrange("p (k f) -> p k f", f=256)
            W2 = sw2.rearrange("p (k c) -> p k c", c=1024)
            a3 = aT.rearrange("p (f n) -> p f n", n=512)
            rwst3 = rwst.rearrange("p (k e) -> p k e", e=NE)
            rwb3 = rwb.rearrange("p (k e) -> p k e", e=NE)
            SELM3 = SELM[:].rearrange("p (t e) -> p t e", e=NE)
            GW3 = GWt[:].rearrange("p (t e) -> p t e", e=NE)
            TOP83 = TOP8[:].rearrange("p (t k) -> p t k", k=8)
            Mall3 = Mall[:].rearrange("p (t e) -> p t e", e=NE)
            for q in range(2):
                ws = wst[q].rearrange("p (k c) -> p k c", c=512)
                P.dma(lambda e, q=q, ws=ws: e.dma_start(
                    out=ws, in_=wout_d[:, q * 512:(q + 1) * 512].rearrange("(k p) c -> p k c", p=128)), writes=["wst%d" % q])
                P.op("dve", lambda e, q=q, ws=ws: e.tensor_copy(out=Wo3[:, :, q * 512:(q + 1) * 512], in_=ws),
                     reads=["wst%d" % q], writes=["Wo"])
            P.dma(lambda e: e.dma_start(out=rwst3, in_=rw_d.rearrange("(k p) e -> p k e", p=128)), writes=["rwst"])
            P.op("dve", lambda e: e.tensor_copy(out=rwb, in_=rwst), reads=["rwst"], writes=["rwb"])
            P.dma(lambda e: e.dma_start(out=sst, in_=ew1_d[NE * 128:(NE + 1) * 128, :]), writes=["sst"])
            P.op("dve", lambda e: e.tensor_copy(out=sw1, in_=sst), reads=["sst"], writes=["sw1"])
            P.dma(lambda e: e.dma_start(out=sst, in_=ew3_d[NE * 128:(NE + 1) * 128, :]), writes=["sst"])
            P.op("dve", lambda e: e.tensor_copy(out=sw3, in_=sst), reads=["sst"], writes=["sw3"])
            P.dma(lambda e: e.dma_start(out=sst, in_=ew2_d[NE * 128:(NE + 1) * 128, :]), writes=["sst"])
            for k2 in range(2):
                P.op("dve", lambda e, k2=k2: e.tensor_tensor(out=sw2[:, k2 * 1024:(k2 + 1) * 1024], in0=sst[:, k2 * 1024:(k2 + 1) * 1024],
                                                             in1=g2bc[:], op=ALU.mult), reads=["sst", "g2bc"], writes=["sw2"])
            sc = rt[:, 0:64]; sel = rt[:, 64:128]; eq = rt[:, 128:192]; sel2 = rt[:, 192:256]
            wts = rt[:, 320:384]
            m1 = rt[:, 384:392]; m2 = rt[:, 392:400]; gs = rt[:, 400:408]; top = rt[:, 408:416]
            gm = rt[:, 416:424]; wsum = rt[:, 432:433]
            v3 = lambda a: a.rearrange("p (g k) -> p g k", k=8)
            st_ = {"ci": 0, "sgi": 0}
            for blk in range(NQB):
                def load(t, xt, xr, blk=blk):
                    row0 = blk * 512 + t * 128
                    ci = st_["ci"]
                    st_["ci"] += 1
                    c = ct[ci % 2]; cn = "ct%d" % (ci % 2)
                    cTt = cT[ci % 2]; cTn = "cT%d" % (ci % 2)
                    P.dma(lambda e: e.dma_start(out=c, in_=CAT[row0:row0 + 128, 0:512]), writes=[cn])
                    cr3 = crT[blk % 2].rearrange("p (j n) -> p j n", n=512)
                    crn = "crT%d" % (blk % 2)
                    if t == 0:
                        P.dma(lambda e: e.dma_start(out=cr3, in_=CATT[:, blk * 512:(blk + 1) * 512].rearrange("(j p) n -> p j n", p=128)),
                              writes=[crn])
                    P.dma(lambda e: e.dma_start(out=xt, in_=xtok[CTX + row0:CTX + row0 + 128, :]), writes=[xr])

                    def tr(e):
                        pv = banks[4][:].bitcast(BF16)
                        for kc in range(4):
                            ins = e.transpose(out=pv[:, kc * 128:(kc + 1) * 128], in_=c[:, kc * 128:(kc + 1) * 128], identity=ident[:])
                        return ins
                    P.op("pe", tr, reads=[cn, "ident"], writes=["b4"])
                    P.op("act", lambda e: e.activation(out=cTt, in_=banks[4][:].bitcast(BF16)[:, 0:512], func=AF.Identity),
                         reads=["b4"], writes=[cTn])

                    def mm(e):
                        for hh in range(2):
                            for kc in range(8):
                                lt = cTt[:, kc * 128:(kc + 1) * 128] if kc < 4 else cr3[:, kc - 4, t * 128:(t + 1) * 128]
                                ins = e.matmul(banks[5 + hh][:], lhsT=lt,
                                               rhs=Wo3[:, kc, hh * 512:(hh + 1) * 512], start=(kc == 0), stop=(kc == 7))
                        return ins
                    P.op("pe", mm, reads=[cTn, crn, "Wo"], writes=["y5", "y6"])
                    for hh in range(2):
                        P.op("dve", lambda e, hh=hh: e.tensor_tensor(out=tmp[:, hh * 512:(hh + 1) * 512], in0=banks[5 + hh][:],
                                                                     in1=g1bc[:, hh * 512:(hh + 1) * 512], op=ALU.mult),
                             reads=["y%d" % (5 + hh), "g1bc"], writes=["tmp%d" % hh])
                    P.op("dve", lambda e: e.tensor_tensor(out=xt, in0=xt, in1=tmp, op=ALU.add), reads=[xr, "tmp0", "tmp1"], writes=[xr])

                def store_xn(t, xn, nr, blk=blk):
                    row0 = blk * 512 + t * 128
                    P.dma(lambda e: e.dma_start(out=XN[row0:row0 + 128, :], in_=xn), reads=[nr], eng="pool")
                norm_block(4, load, xts, xns, junk, hT3, A2, B2, "p4", after_xn=store_xn)
                for t in range(4):
                    tt = blk * 4 + t
                    selm = SELM3[:, tt, :]
                    top8 = TOP83[:, tt, :]

                    def rmm(e, t=t):
                        for kc in range(8):
                            ins = e.matmul(banks[7][:, 0:NE], lhsT=hT3[:, kc, t * 128:(t + 1) * 128], rhs=rwb3[:, kc, :],
                                           start=(kc == 0), stop=(kc == 7))
                        return ins
                    P.op("pe", rmm, reads=["hT_p4", "rwb"], writes=["y7"])
                    P.op("act", lambda e: e.activation(out=sc, in_=banks[7][:, 0:NE], func=AF.Sigmoid), reads=["y7"], writes=["sc"])
                    P.op("dve", lambda e: e.tensor_tensor(out=sel, in0=sc, in1=rbias[:], op=ALU.add), reads=["sc", "rbias"], writes=["sel"])
                    P.op("dve", lambda e: e.tensor_reduce(out=m1, in_=v3(sel), axis=AX.X, op=ALU.max), reads=["sel"], writes=["m1"])
                    P.op("dve", lambda e: e.tensor_tensor(out=v3(eq), in0=v3(sel), in1=m1.unsqueeze(2).to_broadcast([128, 8, 8]),
                                                          op=ALU.is_equal), reads=["sel", "m1"], writes=["eq"])
                    P.op("dve", lambda e: e.scalar_tensor_tensor(out=sel2, in0=eq, scalar=-1e30, in1=sel, op0=ALU.mult, op1=ALU.add),
                         reads=["eq", "sel"], writes=["sel2"])
                    P.op("dve", lambda e: e.tensor_reduce(out=m2, in_=v3(sel2), axis=AX.X, op=ALU.max), reads=["sel2"], writes=["m2"])
                    P.op("dve", lambda e: e.tensor_tensor(out=gs, in0=m1, in1=m2, op=ALU.add), reads=["m1", "m2"], writes=["gs"])
                    P.op("dve", lambda e: e.max(out=top, in_=gs), reads=["gs"], writes=["top"])
                    P.op("dve", lambda e: e.tensor_scalar(out=gm, in0=gs, scalar1=top[:, 3:4], scalar2=None, op0=ALU.is_ge),
                         reads=["gs", "top"], writes=["gm"])
                    P.op("dve", lambda e: e.tensor_scalar(out=gm, in0=gm, scalar1=-1.0, scalar2=1e30, op0=ALU.add, op1=ALU.mult),
                         reads=["gm"], writes=["gm"])
                    P.op("dve", lambda e, selm=selm: e.tensor_tensor(out=v3(selm), in0=v3(sel), in1=gm.unsqueeze(2).to_broadcast([128, 8, 8]),
                                                                     op=ALU.add), reads=["sel", "gm"], writes=["selm%d" % tt])
                    P.op("dve", lambda e, selm=selm, top8=top8: e.max(out=top8, in_=selm), reads=["selm%d" % tt], writes=["top8_%d" % tt])
                    P.op("dve", lambda e, selm=selm, top8=top8: e.tensor_scalar(out=eq, in0=selm, scalar1=top8[:, 7:8], scalar2=None, op0=ALU.is_ge),
                         reads=["selm%d" % tt, "top8_%d" % tt], writes=["eq"])
                    P.op("dve", lambda e, tt=tt: e.tensor_copy(out=Mall3[:, tt, :], in_=eq), reads=["eq"], writes=["Mall"])
                    P.op("dve", lambda e: e.tensor_tensor(out=wts, in0=sc, in1=eq, op=ALU.mult), reads=["sc", "eq"], writes=["wts"])
                    P.op("dve", lambda e: e.tensor_reduce(out=wsum, in_=wts, axis=AX.X, op=ALU.add), reads=["wts"], writes=["wsum"])
                    P.op("dve", lambda e: e.reciprocal(out=wsum, in_=wsum), reads=["wsum"], writes=["wsum"])
                    P.op("dve", lambda e, tt=tt: e.tensor_scalar(out=GW3[:, tt, :], in0=wts, scalar1=wsum, scalar2=2.5,
                                                                 op0=ALU.mult, op1=ALU.mult), reads=["wts", "wsum"], writes=["GW"])
                for fc in range(2):
                    s_ = sg[st_["sgi"] % 2]; sn = "sg%d" % (st_["sgi"] % 2)
                    st_["sgi"] += 1

                    def mm13(e, fc=fc):
                        for kc in range(8):
                            e.matmul(banks[0 + fc][:], lhsT=W1[:, kc, fc * 128:(fc + 1) * 128], rhs=hT3[:, kc, :],
                                     start=(kc == 0), stop=(kc == 7))
                        for kc in range(8):
                            ins = e.matmul(banks[2 + fc][:], lhsT=W3[:, kc, fc * 128:(fc + 1) * 128], rhs=hT3[:, kc, :],
                                           start=(kc == 0), stop=(kc == 7))
                        return ins
                    P.op("pe", mm13, reads=["sw1", "sw3", "hT_p4"], writes=["pb%d" % fc, "pb%d" % (2 + fc)])
                    P.op("act", lambda e, fc=fc, s_=s_: e.activation(out=s_, in_=banks[0 + fc][:], func=AF.Silu),
                         reads=["pb%d" % fc], writes=[sn])
                    P.op("dve", lambda e, fc=fc, s_=s_: e.tensor_tensor(out=a3[:, fc, :], in0=banks[2 + fc][:], in1=s_, op=ALU.mult),
                         reads=["pb%d" % (2 + fc), sn], writes=["aT"])
                for t in range(4):
                    xt = xts[t]
                    xr = "p4_x%d" % t
                    for hh in range(2):
                        bk = 5 + hh

                        def mm2(e, t=t, hh=hh, bk=bk):
                            for fc in range(2):
                                ins = e.matmul(banks[bk][:], lhsT=a3[:, fc, t * 128:(t + 1) * 128],
                                               rhs=W2[:, fc, hh * 512:(hh + 1) * 512], start=(fc == 0), stop=(fc == 1))
                            return ins
                        P.op("pe", mm2, reads=["aT", "sw2"], writes=["y%d" % bk])
                        xv = xt[:, hh * 512:(hh + 1) * 512]
                        P.op("dve", lambda e, bk=bk, xv=xv: e.tensor_tensor(out=xv, in0=banks[bk][:], in1=xv, op=ALU.add),
                             reads=["y%d" % bk, xr], writes=[xr])
                    row0 = blk * 512 + t * 128
                    P.dma(lambda e, xt=xt, row0=row0: e.dma_start(out=X1[row0:row0 + 128, :], in_=xt), reads=[xr], eng="pool")
            P.barrier()

        def phase5():
            I32 = mybir.dt.int32
            NBLK = HALF * 8 // 512 + NE
            NT = HALF // 128
            AR.reset()
            Utri = AR.bf16(128); ones_b = AR.bf16(128)
            J512 = AR.f32(8); B512 = AR.f32(128); iotap = AR.f32(1)
            CNT = AR.f32(64); NBk = AR.f32(64); PADD = AR.f32(64); ca = AR.f32(64); cb = AR.f32(64); PST = AR.f32(64)
            RANK = AR.f32(NT * 64)
            EB = AR.f32(NBLK); WIDXf = AR.f32(NBLK); WIDX = AR.f32(NBLK).bitcast(I32)
            DKf = AR.f32(NT * 8); DKi = AR.f32(NT * 8).bitcast(I32); WK = AR.f32(NT * 8)
            UNU = AR.f32(NBLK); XBASE = AR.f32(NBLK * 4); XIDXf = AR.f32(NBLK * 4); XIDX = AR.f32(NBLK * 4).bitcast(I32)
            regs = {}

            def breg(e, name, val):
                if name not in regs:
                    regs[name] = e.alloc_register(name)
                    e.reg_mov(regs[name], val)
                return regs[name]
            mark = AR.off
            cmp3 = AR.f32(NBLK * 64)
            oh = AR.f32(512); pr = AR.f32(512)
            SELM3 = SELM[:].rearrange("p (t e) -> p t e", e=NE)
            GW3 = GWt[:].rearrange("p (t e) -> p t e", e=NE)
            TOP83 = TOP8[:].rearrange("p (t k) -> p t k", k=8)
            Mall3 = Mall[:].rearrange("p (t e) -> p t e", e=NE)
            RANK3 = RANK.rearrange("p (t e) -> p t e", e=64)
            DKf3 = DKf.rearrange("p (t k) -> p t k", k=8)
            DKi3 = DKi.rearrange("p (t k) -> p t k", k=8)
            WK3 = WK.rearrange("p (t k) -> p t k", k=8)
            for (dst, src, nm) in ((Utri, utri_d, "Utri"), (ones_b, onesb_d, "ones_b"), (J512, j512_d, "J512"),
                                   (B512, b512_d, "B512"), (iotap, iotap_d, "iotap"), (XBASE, xbase_d, "XBASE")):
                P.dma(lambda e, dst=dst, src=src: e.dma_start(out=dst, in_=src), writes=[nm])
            def cnt_mm(e):
                for t in range(NT):
                    ins = e.matmul(banks[0][:, 0:64], lhsT=ones_b, rhs=Mall3[:, t, :], start=(t == 0), stop=(t == NT - 1))
                return ins
            P.op("pe", cnt_mm, reads=["ones_b", "Mall"], writes=["cntps"])
            P.op("dve", lambda e: e.tensor_copy(out=CNT, in_=banks[0][:, 0:64]), reads=["cntps"], writes=["CNT"])

            def rk_mm(e):
                for t in range(NT):
                    dst = banks[1 + t // 8][:, (t % 8) * 64:(t % 8 + 1) * 64]
                    ins = e.matmul(dst, lhsT=Utri, rhs=Mall3[:, t, :], start=True, stop=(t == 0))
                    for t2 in range(t):
                        ins = e.matmul(dst, lhsT=ones_b, rhs=Mall3[:, t2, :], start=False, stop=(t2 == t - 1))
                return ins
            P.op("pe", rk_mm, reads=["ones_b", "Utri", "Mall"], writes=["rkps"])
            for b4 in range(4):
                P.op("dve", lambda e, b4=b4: e.tensor_copy(out=RANK[:, b4 * 512:(b4 + 1) * 512], in_=banks[1 + b4][:]),
                     reads=["rkps"], writes=["RANK"])
            t8 = cmp3[:, 0:512].rearrange("p (e j) -> p e j", j=8)
            P.op("dve", lambda e: e.tensor_tensor(out=t8, in0=CNT.unsqueeze(2).to_broadcast([128, 64, 8]),
                                                  in1=J512.unsqueeze(1).to_broadcast([128, 64, 8]), op=ALU.is_gt),
                 reads=["CNT", "J512"], writes=["t8"])
            P.op("dve", lambda e: e.tensor_reduce(out=NBk, in_=t8, axis=AX.X, op=ALU.add), reads=["t8"], writes=["NBk"])
            P.op("dve", lambda e: e.tensor_scalar(out=PADD, in0=NBk, scalar1=512.0, scalar2=None, op0=ALU.mult), reads=["NBk"], writes=["PADD"])
            P.op("dve", lambda e: e.tensor_copy(out=ca, in_=PADD), reads=["PADD"], writes=["ca"])
            cur, nxt, cn_, nn_ = ca, cb, "ca", "cb"
            for sft in (1, 2, 4, 8, 16, 32):
                P.op("dve", lambda e, cur=cur, nxt=nxt, sft=sft: e.tensor_copy(out=nxt[:, 0:sft], in_=cur[:, 0:sft]), reads=[cn_], writes=[nn_])
                P.op("dve", lambda e, cur=cur, nxt=nxt, sft=sft: e.tensor_tensor(out=nxt[:, sft:64], in0=cur[:, sft:64], in1=cur[:, 0:64 - sft], op=ALU.add),
                     reads=[cn_, nn_], writes=[nn_])
                cur, nxt, cn_, nn_ = nxt, cur, nn_, cn_
            PEND, pend_n = cur, cn_
            P.op("dve", lambda e: e.tensor_tensor(out=PST, in0=PEND, in1=PADD, op=ALU.subtract), reads=[pend_n, "PADD"], writes=["PST"])
            P.op("dve", lambda e: e.tensor_tensor(out=RANK3, in0=RANK3, in1=PST.unsqueeze(1).to_broadcast([128, NT, 64]), op=ALU.add),
                 reads=["RANK", "PST"], writes=["RANK"])
            c3 = cmp3.rearrange("p (b e) -> p b e", e=64)
            P.op("dve", lambda e: e.tensor_tensor(out=c3, in0=PEND.unsqueeze(1).to_broadcast([128, NBLK, 64]),
                                                  in1=B512.unsqueeze(2).to_broadcast([128, NBLK, 64]), op=ALU.is_le),
                 reads=[pend_n, "B512", "t8"], writes=["c3"])
            P.op("dve", lambda e: e.tensor_reduce(out=EB, in_=c3, axis=AX.X, op=ALU.add), reads=["c3"], writes=["EB"])
            P.op("dve", lambda e: e.tensor_scalar(out=WIDXf, in0=EB, scalar1=128.0, scalar2=iotap[:, 0:1], op0=ALU.mult, op1=ALU.add),
                 reads=["EB", "iotap"], writes=["WIDXf"])
            P.op("dve", lambda e: e.tensor_copy(out=WIDX, in_=WIDXf), reads=["WIDXf"], writes=["WIDX"])
            P.op("dve", lambda e: e.tensor_scalar(out=UNU, in0=EB, scalar1=float(NE), scalar2=1.0e6, op0=ALU.is_ge, op1=ALU.mult),
                 reads=["EB"], writes=["UNU"])
            P.op("dve", lambda e: e.tensor_tensor(out=XIDXf.rearrange("p (b s) -> p b s", s=4), in0=XBASE.rearrange("p (b s) -> p b s", s=4),
                                                  in1=UNU.unsqueeze(2).to_broadcast([128, NBLK, 4]), op=ALU.add),
                 reads=["UNU", "XBASE"], writes=["XIDXf"])
            P.op("dve", lambda e: e.tensor_copy(out=XIDX, in_=XIDXf), reads=["XIDXf"], writes=["XIDX"])
            oh3 = oh.rearrange("p (k e) -> p k e", e=64)
            pr3 = pr.rearrange("p (k e) -> p k e", e=64)
            for t in range(NT):
                P.op("dve", lambda e, t=t: e.tensor_tensor(out=oh3, in0=SELM3[:, t, :].unsqueeze(1).to_broadcast([128, 8, 64]),
                                                           in1=TOP83[:, t, :].unsqueeze(2).to_broadcast([128, 8, 64]), op=ALU.is_equal),
                     reads=["c3"], writes=["oh"])
                P.op("dve", lambda e, t=t: e.tensor_tensor(out=pr3, in0=oh3, in1=RANK3[:, t, :].unsqueeze(1).to_broadcast([128, 8, 64]), op=ALU.mult),
                     reads=["oh", "RANK"], writes=["pr"])
                P.op("dve", lambda e, t=t: e.tensor_reduce(out=DKf3[:, t, :], in_=pr3, axis=AX.X, op=ALU.add), reads=["pr"], writes=["DKf"])
                P.op("dve", lambda e, t=t: e.tensor_tensor(out=pr3, in0=oh3, in1=GW3[:, t, :].unsqueeze(1).to_broadcast([128, 8, 64]), op=ALU.mult),
                     reads=["oh", "DKf"], writes=["pr"])
                P.op("dve", lambda e, t=t: e.tensor_reduce(out=WK3[:, t, :], in_=pr3, axis=AX.X, op=ALU.add), reads=["pr"], writes=["WK"])
            P.op("dve", lambda e: e.tensor_scalar(out=DKf, in0=DKf, scalar1=float(NBLK * 512 - 1), scalar2=0.0, op0=ALU.min, op1=ALU.max),
                 reads=["DKf"], writes=["DKf"])
            P.op("dve", lambda e: e.tensor_copy(out=DKi, in_=DKf), reads=["DKf"], writes=["DKi"])
            if debug:
                P.dma(lambda e: e.dma_start(out=dbg_dk, in_=DKf), reads=["DKf"])
                P.dma(lambda e: e.dma_start(out=dbg_wk, in_=WK), reads=["WK"])
                P.dma(lambda e: e.dma_start(out=dbg_eb, in_=EB), reads=["EB"])
            P.barrier()
            AR.off = mark
            xin = [AR.bf16(1024) for _ in range(4)]
            for t in range(NT):
                xi = xin[t % 4]; xn_ = "xin%d" % (t % 4)
                P.dma(lambda e, xi=xi, t=t: e.dma_start(out=xi, in_=XN[t * 128:(t + 1) * 128, :]), writes=[xn_])
                for k in range(8):
                    P.dma(lambda e, xi=xi, t=t, k=k: e.indirect_dma_start(
                        out=XS[:, :], out_offset=bass.IndirectOffsetOnAxis(ap=DKi3[:, t, k:k + 1], axis=0),
                        in_=xi, in_offset=None),
                        reads=[xn_, "DKi", "XSzero"], writes=["XS_%d_%d" % (t, k)], eng="pool")
            P.barrier()
            AR.off = mark
            w1b = [AR.bf16(2048), AR.bf16(2048)]; w3b = [AR.bf16(2048), AR.bf16(2048)]; w2b = [AR.bf16(2048), AR.bf16(2048)]
            xsb = [AR.bf16(4096), AR.bf16(4096)]
            xsTb = [AR.bf16(8 * 512), AR.bf16(8 * 512)]
            sg = [AR.f32(512), AR.f32(512)]
            aT = AR.bf16(1024)
            ysb = [AR.f32(4096), AR.f32(4096)]
            a3 = aT.rearrange("p (f n) -> p f n", n=512)
            st_ = {"sgi": 0, "evi": 0}

            def weights(b):
                wb = b % 2
                for (dst_, tab, nm) in ((w1b[wb], EWB[0], "w1b%d" % wb), (w3b[wb], EWB[1], "w3b%d" % wb), (w2b[wb], EWB[2], "w2b%d" % wb)):
                    P.dma(lambda e, dst_=dst_, tab=tab, b=b: e.indirect_dma_start(
                        out=dst_, out_offset=None, in_=tab[:, :],
                        in_offset=bass.IndirectOffsetOnAxis(ap=WIDX[:, b:b + 1], axis=0),
                        bounds_check=breg(e, "bcw", NE * 128 - 1), oob_is_err=False),
                        reads=["WIDX"], writes=[nm], eng="pool")

            def casts(b):
                pass

            def xs_load(b):
                xs3 = xsb[b % 2].rearrange("p (s c) -> p s c", c=1024)
                P.dma(lambda e, xs3=xs3, b=b: e.dma_start(out=xs3, in_=XS[b * 512:(b + 1) * 512, :].rearrange("(s p) c -> p s c", p=128)),
                      reads=["XS"], writes=["xsb%d_%d" % (b % 2, i4) for i4 in range(4)])

            def prep_half(b, half):
                xs3 = xsb[b % 2].rearrange("p (s c) -> p s c", c=1024)
                xsT3 = xsTb[b % 2].rearrange("p (k n) -> p k n", n=512)
                xtn = "xsT%d" % (b % 2)

                def tr(e, xs3=xs3, half=half):
                    for s4 in range(4):
                        for kk in range(4):
                            kc = half * 4 + kk
                            pv = banks[kk // 2][:].bitcast(BF16)
                            ins = e.transpose(out=pv[:, (kk % 2) * 512 + s4 * 128:(kk % 2) * 512 + (s4 + 1) * 128],
                                              in_=xs3[:, s4, kc * 128:(kc + 1) * 128], identity=ident[:])
                    return ins
                P.op("pe", tr, reads=["xsb%d_%d" % (b % 2, i4) for i4 in range(4)] + ["ident"], writes=["pb0", "pb1"])
                for kk in range(4):
                    kc = half * 4 + kk
                    pv = banks[kk // 2][:].bitcast(BF16)
                    src = pv[:, (kk % 2) * 512:(kk % 2) * 512 + 512]
                    if st_["evi"] % 2 == 0:
                        P.op("act", lambda e, kc=kc, src=src, xsT3=xsT3: e.activation(
                            out=xsT3[:, kc, :], in_=src, func=AF.Identity, scale=A2[:, kc:kc + 1], bias=B2[:, kc:kc + 1]),
                            reads=["pb%d" % (kk // 2)], writes=[xtn])
                    else:
                        P.op("dve", lambda e, kc=kc, src=src, xsT3=xsT3: e.tensor_scalar(
                            out=xsT3[:, kc, :], in0=src, scalar1=A2[:, kc:kc + 1], scalar2=B2[:, kc:kc + 1], op0=ALU.mult, op1=ALU.add),
                            reads=["pb%d" % (kk // 2)], writes=[xtn])
                    st_["evi"] += 1

            def up_gate(b, fc):
                wb = b % 2
                W1 = w1b[wb].rearrange("p (k f) -> p k f", f=256)
                W3 = w3b[wb].rearrange("p (k f) -> p k f", f=256)
                xsT3 = xsTb[b % 2].rearrange("p (k n) -> p k n", n=512)
                bu, bg = (2, 3) if fc == 0 else (4, 5)
                s_ = sg[st_["sgi"] % 2]; sn = "sg%d" % (st_["sgi"] % 2)
                st_["sgi"] += 1

                def mm13(e):
                    for kc in range(8):
                        e.matmul(banks[bu][:], lhsT=W1[:, kc, fc * 128:(fc + 1) * 128], rhs=xsT3[:, kc, :], start=(kc == 0), stop=(kc == 7))
                    for kc in range(8):
                        ins = e.matmul(banks[bg][:], lhsT=W3[:, kc, fc * 128:(fc + 1) * 128], rhs=xsT3[:, kc, :], start=(kc == 0), stop=(kc == 7))
                    return ins
                P.op("pe", mm13, reads=["w1b%d" % wb, "w3b%d" % wb, "xsT%d" % (b % 2)], writes=["pb%d" % bu, "pb%d" % bg])
                P.op("act", lambda e: e.activation(out=s_, in_=banks[bu][:], func=AF.Silu), reads=["pb%d" % bu], writes=[sn])
                P.op("dve", lambda e: e.tensor_tensor(out=a3[:, fc, :], in0=banks[bg][:], in1=s_, op=ALU.mult),
                     reads=["pb%d" % bg, sn], writes=["aT"])

            def down(b):
                wb = b % 2
                W2 = w2b[wb].rearrange("p (k c) -> p k c", c=1024)
                ys_ = ysb[b % 2]; ysn = "ysb%d" % (b % 2)
                ys3 = ys_.rearrange("p (s c) -> p s c", c=1024)
                ybanks = [6, 7, 2, 3]
                for s4 in range(4):
                    for hh in range(2):
                        bk = ybanks[(s4 * 2 + hh) % 4]

                        def mm2(e, s4=s4, hh=hh, bk=bk):
                            for fc in range(2):
                                ins = e.matmul(banks[bk][:], lhsT=a3[:, fc, s4 * 128:(s4 + 1) * 128],
                                               rhs=W2[:, fc, hh * 512:(hh + 1) * 512], start=(fc == 0), stop=(fc == 1))
                            return ins
                        P.op("pe", mm2, reads=["aT", "w2b%d" % wb], writes=["pb%d" % bk])
                        yv = ys3[:, s4, hh * 512:(hh + 1) * 512]
                        if (s4 * 2 + hh) % 2 == 0:
                            P.op("act", lambda e, bk=bk, yv=yv: e.activation(out=yv, in_=banks[bk][:], func=AF.Identity),
                                 reads=["pb%d" % bk], writes=[ysn])
                        else:
                            P.op("dve", lambda e, bk=bk, yv=yv: e.tensor_copy(out=yv, in_=banks[bk][:]), reads=["pb%d" % bk], writes=[ysn])
                P.dma(lambda e, ys3=ys3, b=b: e.dma_start(out=YS[b * 512:(b + 1) * 512, :].rearrange("(s p) c -> p s c", p=128), in_=ys3),
                      reads=[ysn], writes=["YS_%d" % b])

            weights(0)
            xs_load(0)
            weights(1)
            xs_load(1)
            casts(0)
            prep_half(0, 0)
            prep_half(0, 1)
            for b in range(NBLK):
                nb_ = b + 1
                if b + 2 < NBLK:
                    xs_load(b + 2)
                up_gate(b, 0)
                if nb_ < NBLK:
                    prep_half(nb_, 0)
                up_gate(b, 1)
                if nb_ < NBLK:
                    prep_half(nb_, 1)
                down(b)
                if b + 2 < NBLK:
                    weights(b + 2)
            P.barrier()
            AR.off = mark
            accb = [AR.f32(1024), AR.f32(1024)]
            rows = [AR.f32(1024) for _ in range(8)]
            junk = AR.bf16(1024)
            obuf = [AR.f32(1024), AR.f32(1024)]
            ri = 0
            accb.append(AR.f32(1024))
            P.dma(lambda e: e.dma_start(out=accb[0], in_=X1[0:128, :]), writes=["acc0"])
            for t in range(NT):
                acc = accb[t % 3]; an = "acc%d" % (t % 3)
                if t + 1 < NT:
                    P.dma(lambda e, t=t: e.dma_start(out=accb[(t + 1) % 3], in_=X1[(t + 1) * 128:(t + 2) * 128, :]),
                          writes=["acc%d" % ((t + 1) % 3)])
                for k in range(8):
                    rw = rows[ri % 8]; rn = "row%d" % (ri % 8)
                    ri += 1
                    P.dma(lambda e, rw=rw, t=t, k=k: e.indirect_dma_start(
                        out=rw, out_offset=None, in_=YS[:, :],
                        in_offset=bass.IndirectOffsetOnAxis(ap=DKi3[:, t, k:k + 1], axis=0)),
                        reads=["YS", "DKi"], writes=[rn], eng="pool")
                    P.op("dve", lambda e, rw=rw, acc=acc, t=t, k=k: e.scalar_tensor_tensor(
                        out=acc, in0=rw, scalar=WK3[:, t, k:k + 1], in1=acc, op0=ALU.mult, op1=ALU.add),
                        reads=[rn, an, "WK"], writes=[an])
                ob = obuf[t % 2]; obn = "obuf%d" % (t % 2)
                ssq = stat[:, 48 + (t % 2):49 + (t % 2)]
                sn = "fss%d" % (t % 2)
                P.op("act", lambda e, acc=acc, ssq=ssq: e.activation(out=junk, in_=acc, func=AF.Square, accum_out=ssq),
                     reads=[an], writes=["junk", sn])
                P.op("dve", lambda e, ssq=ssq: e.tensor_scalar(out=ssq, in0=ssq, scalar1=1.0 / D, scalar2=EPS, op0=ALU.mult, op1=ALU.add),
                     reads=[sn], writes=[sn])
                P.op("act", lambda e, ssq=ssq: e.activation(out=ssq, in_=ssq, func=AF.Sqrt), reads=[sn], writes=[sn])
                P.op("dve", lambda e, ssq=ssq: e.reciprocal(out=ssq, in_=ssq), reads=[sn], writes=[sn])
                P.op("dve", lambda e, acc=acc, ob=ob, ssq=ssq: e.scalar_tensor_tensor(
                    out=ob, in0=acc, scalar=ssq, in1=fgbc[:], op0=ALU.mult, op1=ALU.mult),
                    reads=[an, sn, "fgbc"], writes=[obn])
                P.dma(lambda e, ob=ob, t=t: e.dma_start(out=y_d[t * 128:(t + 1) * 128, :], in_=ob), reads=[obn])
            P.barrier()

        LN8c = sb("LN8c", [128, 1])
        EPSc = sb("EPSc", [128, 1])
        P.op("dve", lambda e: e.memset(LN8c[:], LN8), writes=["LN8c"])
        P.op("dve", lambda e: e.memset(EPSc[:], EPS), writes=["EPSc"])
        phase0()
        if upto >= 1:
            phase1()
        if upto >= 2:
            phase2_attention()
        if upto >= 3:
            phase3_retention()
        if upto >= 4:
            phase4()
        if upto >= 5:
            phase5()
        P.barrier()
        P.emit()
    return nc


def _rope_tables():
    n_freq = 16
    inv = (10000.0 ** (-np.arange(n_freq, dtype=np.float32) / n_freq)).astype(np.float32)
    t = np.arange(SEQ)
    rows = (t // 64).astype(np.float32)
    cols = (t % 64).astype(np.float32)
    ang = np.stack([rows[:, None] * inv[None], cols[:, None] * inv[None]], axis=1).astype(np.float32)
    c = np.cos(ang).astype(np.float32)
    s = np.sin(ang).astype(np.float32)
    C = np.zeros((64, SEQ), np.float32)
    S = np.zeros((64, SEQ), np.float32)
    for ax in range(2):
        for hf in range(2):
            C[ax * 32 + hf * 16:ax * 32 + hf * 16 + 16] = c[:, ax, :].T
            S[ax * 32 + hf * 16:ax * 32 + hf * 16 + 16] = s[:, ax, :].T
    return np.concatenate([C, C], 0), np.concatenate([S, S], 0)


def _const_tables():
    j = np.arange(128, dtype=np.float32)[:, None]
    i = np.arange(512, dtype=np.float32)[None, :]
    idxe = (i - j).astype(np.float32)
    df = []
    db = []
    for s in range(4):
        a = i - 128 * s - j
        df.append(np.where(a >= 0, a, BIG))
        b = 128 * s + j - i
        db.append(np.where(b >= 0, b, BIG))
    diagf = np.concatenate(df, 1).astype(np.float32)
    diagb = np.concatenate(db, 1).astype(np.float32)
    iota = np.tile((128.0 * np.arange(32, dtype=np.float32))[None], (128, 1))
    return idxe, diagf, diagb, iota


def _offsets(hf):
    offo = np.zeros((NQB, 32), np.float32)
    offcf = np.zeros((NQB, 2), np.float32)
    offcb = np.zeros((NQB, 2), np.float32)
    for qb in range(NQB):
        n0 = hf * HALF + 512 * qb
        for cc in range(2):
            c0 = 128 * cc
            offcf[qb, cc] = n0 + 256 - c0
            offcb[qb, cc] = SEQ - n0 + c0
        for ko in range(32):
            m0 = (1 - hf) * HALF + 128 * ko
            offo[qb, ko] = abs(n0 - m0)
    t = lambda a: np.tile(a.reshape(1, -1), (128, 1)).astype(np.float32)
    selv = np.tile(np.array([[1.0, 0.0]] if hf == 1 else [[0.0, 1.0]], np.float32), (128, 1))
    return t(offo), t(offcf), t(offcb), selv


def make_in_maps(inp):
    f = lambda a: np.ascontiguousarray(np.asarray(a, dtype=np.float32))
    x = f(inp["x"]); c = f(inp["c"]); ctx = f(inp["ctx"]); c_ctx = f(inp["c_ctx"])
    C, S = _rope_tables()
    idxe, diagf, diagb, iota = _const_tables()
    ident = np.eye(128, dtype=np.float32).astype(ml_dtypes.bfloat16)
    fm = lambda v: np.ascontiguousarray(v.reshape(8, 128).T)
    def lay13(w):
        E = w.shape[0]
        return np.ascontiguousarray(w.reshape(E, 8, 128, 256).transpose(0, 2, 1, 3).reshape(E * 128, 2048))

    def lay2(w):
        E = w.shape[0]
        return np.ascontiguousarray(w.reshape(E, 2, 128, 1024).transpose(0, 2, 1, 3).reshape(E * 128, 2048))
    e_w1 = lay13(np.concatenate([f(inp["exp_w1"])[0], f(inp["shared_w1"])], 0))
    e_w3 = lay13(np.concatenate([f(inp["exp_w3"])[0], f(inp["shared_w3"])], 0))
    e_w2 = lay2(np.concatenate([f(inp["exp_w2"])[0], f(inp["shared_w2"])], 0))
    kk = np.arange(128)
    utri = (kk[:, None] < kk[None, :]).astype(np.float32).astype(ml_dtypes.bfloat16)
    onesb = np.ones((128, 128), np.float32).astype(ml_dtypes.bfloat16)
    j512 = np.tile((512.0 * np.arange(8, dtype=np.float32))[None], (128, 1))
    b512 = np.tile((512.0 * np.arange(128, dtype=np.float32))[None], (128, 1))
    iotap = np.arange(128, dtype=np.float32).reshape(128, 1)
    xbase = (128.0 * np.arange(512, dtype=np.float32)[None, :] + np.arange(128, dtype=np.float32)[:, None]).astype(np.float32)
    lamv = np.concatenate([f(inp["lambda_q1"])[0], f(inp["lambda_q2"])[0], f(inp["lambda_k1"])[0], f(inp["lambda_k2"])[0]])
    rdec = np.concatenate([f(inp["ret_decay_fwd"])[0], f(inp["ret_decay_bwd"])[0]])
    shared = {
        "ada_w": f(inp["ada_w"])[0], "ada_b": f(inp["ada_b"]), "n1g": fm(f(inp["norm1_g"])[0]), "n2g": fm(f(inp["norm2_g"])[0]),
        "w_in": f(inp["w_in"])[0], "w_out": f(inp["w_out"])[0], "lamv": lamv, "dng": f(inp["dattn_norm_g"])[0],
        "rng": f(inp["ret_norm_g"])[0], "rdec": rdec, "router_w": f(inp["router_w"])[0], "router_b": f(inp["router_bias"])[0],
        "e_w1": e_w1, "e_w3": e_w3, "e_w2": e_w2, "fng": f(inp["final_norm_g"]), "ident": ident,
        "idxe": idxe, "diagf": diagf, "diagb": diagb, "iota128": iota,
        "utri": utri, "onesb": onesb, "j512": j512, "b512": b512, "iotap": iotap, "xbase": xbase,
        "ioti": np.tile(np.arange(512, dtype=np.float32)[None], (128, 1)),
    }
    maps = []
    one = np.ones((128, CTX), np.float32)
    zero = np.zeros((128, CTX), np.float32)
    for core in range(8):
        b, hf = core // 2, core % 2
        own = slice(hf * HALF, (hf + 1) * HALF)
        oth = slice((1 - hf) * HALF, (2 - hf) * HALF)
        m = dict(shared)
        m["xtok"] = np.ascontiguousarray(np.concatenate([ctx[b], x[b, own], x[b, oth]], 0))
        c2 = np.stack([fm(c[b]), fm(c_ctx)], axis=2).reshape(128, 16)
        m["c2"] = np.ascontiguousarray(c2)
        m["ropeC"] = np.ascontiguousarray(np.concatenate([one, C[:, own], C[:, oth]], 1))
        m["ropeS"] = np.ascontiguousarray(np.concatenate([zero, S[:, own], S[:, oth]], 1))
        m["offo"], m["offcf"], m["offcb"], m["selv"] = _offsets(hf)
        maps.append(m)
    return maps


def kernel(**inputs):
    nc = build_program()
    in_maps = make_in_maps(inputs)
    res = run_bass_kernel_spmd(nc, in_maps, core_ids=list(range(8)))
    out = np.zeros((4, SEQ, D), np.float32)
    for core in range(8):
        b, hf = core // 2, core % 2
        out[b, hf * HALF:(hf + 1) * HALF] = np.asarray(res.results[core]["y"], dtype=np.float32)
    return out
```
